# Optimizing a Trainium2 kernel written in Bass

```python
import jax
import jax.numpy as jnp
from jax import lax
import numpy as np

D_MODEL = 2048
BATCH = 8
SEQ = 2048
DEPTH = 2
DEC_BATCH = 128
DEC_SEQ = 4
PAST_LEN = 8192
PAGE_SIZE = 128

GLA_HEADS = 4
GLA_DK = D_MODEL // 2
GLA_DV = D_MODEL
GLA_HEAD_K = GLA_DK // GLA_HEADS
GLA_HEAD_V = GLA_DV // GLA_HEADS
GATE_RANK = 16
GATE_TEMP = 16.0
GLA_CHUNK = 64
SWA_HD = 64
SWA_HQ = D_MODEL // SWA_HD
SWA_KV = SWA_HQ // 8
SWA_GROUP = SWA_HQ // SWA_KV
WINDOW = 128
PEER_HEADS = 8
PEER_QDIM = 256
PEER_HALF = PEER_QDIM // 2
N_KEYS = 128
N_EXPERTS = N_KEYS * N_KEYS
PEER_TOPK = 16
PEER_BLOCK = 128
RMS_EPS = 1e-6
IN_WIDTHS = (GLA_DK, GLA_DK, GLA_DV, GLA_DV, GATE_RANK,
             SWA_HQ * SWA_HD, SWA_KV * SWA_HD, SWA_KV * SWA_HD,
             D_MODEL, D_MODEL)
IN_WIDTH = sum(IN_WIDTHS)

kernel_name = 'hybrid_gla_swa_peer_adaln_step'


def rmsnorm(x, g):
    xf = x.astype(jnp.float32)
    xf = xf * lax.rsqrt(jnp.mean(xf * xf, axis=-1, keepdims=True) + RMS_EPS)
    return xf.astype(x.dtype) * g


def gla_scan(q, k, v, log_a, s0, chunk):
    B, L, H, _ = q.shape
    n = L // chunk

    def chunks(t):
        return t.astype(jnp.float32).reshape(B, n, chunk, H, t.shape[-1]).transpose(1, 0, 3, 2, 4)

    causal = jnp.tril(jnp.ones((chunk, chunk), dtype=bool))

    def step(s, inp):
        qc, kc, vc, gc = inp
        b = lax.cumsum(gc, axis=2)
        rel = jnp.where(causal[:, :, None], b[:, :, :, None, :] - b[:, :, None, :, :], -jnp.inf)
        att = jnp.einsum('bhcd,bhsd,bhcsd->bhcs', qc, kc, jnp.exp(rel))
        o = jnp.einsum('bhcd,bhde->bhce', qc * jnp.exp(b), s) + jnp.einsum('bhcs,bhse->bhce', att, vc)
        b_end = b[:, :, -1:, :]
        s_new = s * jnp.exp(b_end)[:, :, 0, :, None] + jnp.einsum('bhsd,bhse->bhde', kc * jnp.exp(b_end - b), vc)
        return s_new, o

    s_fin, o = lax.scan(step, s0, (chunks(q), chunks(k), chunks(v), chunks(log_a)))
    o = o.transpose(1, 0, 3, 2, 4).reshape(B, L, H, v.shape[-1])
    return o, s_fin


def sink_attention(q, k, v, mask, sinks):
    s = jnp.einsum('...qkgd,...skd->...kgqs', q, k).astype(jnp.float32) * (SWA_HD ** -0.5)
    s = jnp.where(mask, s, -jnp.inf)
    sink = sinks.astype(jnp.float32).reshape(SWA_KV, SWA_GROUP, 1, 1)
    m = jnp.maximum(jnp.max(s, axis=-1, keepdims=True), sink)
    p = jnp.exp(s - m)
    p = p / (jnp.sum(p, axis=-1, keepdims=True) + jnp.exp(sink - m))
    return jnp.einsum('...kgqs,...skd->...qkgd', p.astype(v.dtype), v)


def swa_banded(q, k, v, sinks):
    B, L = q.shape[0], q.shape[1]
    n = L // WINDOW
    qb = q.reshape(B, n, WINDOW, SWA_KV, SWA_GROUP, SWA_HD).transpose(1, 0, 2, 3, 4, 5)

    def band(t):
        tb = t.reshape(B, n, WINDOW, SWA_KV, SWA_HD).transpose(1, 0, 2, 3, 4)
        prev = jnp.concatenate([jnp.zeros_like(tb[:1]), tb[:-1]], axis=0)
        return jnp.concatenate([prev, tb], axis=2)

    kb, vb = band(k), band(v)
    d = (jnp.arange(WINDOW)[:, None] + WINDOW) - jnp.arange(2 * WINDOW)[None, :]
    base = (d >= 0) & (d < WINDOW)
    real = (jnp.arange(n)[:, None, None] > 0) | (jnp.arange(2 * WINDOW)[None, None, :] >= WINDOW)
    masks = base[None] & real
    o = lax.map(lambda a: sink_attention(a[0], a[1], a[2], a[3], sinks), (qb, kb, vb, masks))
    return o.transpose(1, 0, 2, 3, 4, 5).reshape(B, L, SWA_HQ * SWA_HD)


def token_mixer(h, gla_s0, swa_k_past, swa_v_past, w_in, w_alpha2, b_alpha, gla_norm_g, sinks, w_out):
    B, L, _ = h.shape
    f32 = jnp.float32
    gq, gk, gv, gr, glr, sq, sk, sv, ga, gb = jnp.split(h @ w_in, np.cumsum(IN_WIDTHS)[:-1].tolist(), axis=-1)

    log_a = jax.nn.log_sigmoid((glr @ w_alpha2 + b_alpha).astype(f32)) / GATE_TEMP
    hk = lambda t: t.reshape(B, L, GLA_HEADS, GLA_HEAD_K)
    if gla_s0 is None:
        s0 = jnp.zeros((B, GLA_HEADS, GLA_HEAD_K, GLA_HEAD_V), f32)
        chunk = min(GLA_CHUNK, L)
    else:
        s0 = gla_s0.astype(f32)
        chunk = L
    o_gla, s_gla = gla_scan(hk(gq) * (GLA_HEAD_K ** -0.5), hk(gk), gv.reshape(B, L, GLA_HEADS, GLA_HEAD_V),
                            hk(log_a), s0, chunk)
    o_gla = rmsnorm(o_gla, gla_norm_g.astype(f32)) * jax.nn.silu(gr.reshape(B, L, GLA_HEADS, GLA_HEAD_V).astype(f32))
    o_gla = o_gla.astype(h.dtype).reshape(B, L, GLA_DV)

    q = sq.reshape(B, L, SWA_KV, SWA_GROUP, SWA_HD)
    k = sk.reshape(B, L, SWA_KV, SWA_HD)
    v = sv.reshape(B, L, SWA_KV, SWA_HD)
    if swa_k_past is None:
        o_swa = swa_banded(q, k, v, sinks)
        k_buf, v_buf = k[:, -WINDOW:], v[:, -WINDOW:]
    else:
        P = swa_k_past.shape[1]
        kk = jnp.concatenate([swa_k_past.astype(k.dtype), k], axis=1)
        vv = jnp.concatenate([swa_v_past.astype(v.dtype), v], axis=1)
        d = jnp.arange(L)[:, None] - jnp.arange(-P, L)[None, :]
        mask = (d >= 0) & (d < WINDOW)
        o_swa = sink_attention(q, kk, vv, mask, sinks).reshape(B, L, SWA_HQ * SWA_HD)
        k_buf, v_buf = kk[:, -P:], vv[:, -P:]

    merged = jax.nn.sigmoid(ga) * o_gla + jax.nn.sigmoid(gb) * o_swa
    return merged @ w_out, s_gla.astype(h.dtype), k_buf, v_buf


def peer_ffn(h, wq, sub_keys, u, v):
    lead = h.shape[:-1]
    x = h.reshape(-1, D_MODEL)
    T = x.shape[0]
    q = (x @ wq).astype(jnp.float32).reshape(T, PEER_HEADS, 2, PEER_HALF)
    s = jnp.einsum('thcd,hcnd->thcn', q, sub_keys.astype(jnp.float32))
    top_s, top_i = lax.top_k(s, PEER_TOPK)
    comb = (top_s[:, :, 0, :, None] + top_s[:, :, 1, None, :]).reshape(T, PEER_HEADS, PEER_TOPK * PEER_TOPK)
    best_s, best_j = lax.top_k(comb, PEER_TOPK)
    i1 = jnp.take_along_axis(top_i[:, :, 0], best_j // PEER_TOPK, axis=-1)
    i2 = jnp.take_along_axis(top_i[:, :, 1], best_j % PEER_TOPK, axis=-1)
    idx = (i1 * N_KEYS + i2).reshape(T, PEER_HEADS * PEER_TOPK)
    gate = jax.nn.softmax(best_s, axis=-1).reshape(T, PEER_HEADS * PEER_TOPK).astype(h.dtype)
    n_blk = -(-T // PEER_BLOCK)
    pad = n_blk * PEER_BLOCK - T

    def blocks(t):
        t = jnp.pad(t, ((0, pad),) + ((0, 0),) * (t.ndim - 1))
        return t.reshape((n_blk, PEER_BLOCK) + t.shape[1:])

    def expert_block(args):
        xb, ib, gblk = args
        act = jax.nn.gelu(jnp.einsum('td,ted->te', xb, jnp.take(u, ib, axis=0)), approximate=False)
        return jnp.einsum('te,ted->td', gblk * act, jnp.take(v, ib, axis=0))

    out = lax.map(expert_block, (blocks(x), blocks(idx), blocks(gate)))
    return out.reshape(n_blk * PEER_BLOCK, D_MODEL)[:T].reshape(lead + (D_MODEL,))


def decoder_layer(x, c, gla_s0, swa_k_past, swa_v_past, w_ada, b_ada, norm1_g, norm2_g, w_in, w_alpha2,
                  b_alpha, gla_norm_g, sinks, w_out, peer_wq, peer_keys, peer_u, peer_v):
    mod = (jax.nn.silu(c) @ w_ada + b_ada)[:, None, :]
    sh1, sc1, gt1, sh2, sc2, gt2 = jnp.split(mod, 6, axis=-1)
    h = rmsnorm(x, norm1_g) * (1 + sc1) + sh1
    y, s_gla, k_buf, v_buf = token_mixer(h, gla_s0, swa_k_past, swa_v_past, w_in, w_alpha2, b_alpha,
                                         gla_norm_g, sinks, w_out)
    x = x + gt1 * y
    h = rmsnorm(x, norm2_g) * (1 + sc2) + sh2
    x = x + gt2 * peer_ffn(h, peer_wq, peer_keys, peer_u, peer_v)
    return x, s_gla, k_buf, v_buf


def setup_inputs(seed: int = 0) -> dict:
    key = jax.random.key(seed)
    ks = jax.random.split(key, 24)
    f32 = jnp.float32
    nrm = lambda k, shape, s: jax.random.normal(k, shape, f32) * s
    win_buf = min(WINDOW, PAST_LEN)
    return {
        'x_prompt': nrm(ks[0], (BATCH, SEQ, D_MODEL), 1.0),
        'x_sample': nrm(ks[1], (DEC_BATCH, DEC_SEQ, D_MODEL), 1.0),
        'state_gla': nrm(ks[2], (DEPTH, DEC_BATCH, GLA_HEADS, GLA_HEAD_K, GLA_HEAD_V), 1.0),
        'cache_swa_k': nrm(ks[3], (DEPTH, DEC_BATCH, win_buf, SWA_KV, SWA_HD), 1.0),
        'cache_swa_v': nrm(ks[4], (DEPTH, DEC_BATCH, win_buf, SWA_KV, SWA_HD), 1.0),
        'c_prompt': nrm(ks[5], (BATCH, D_MODEL), 1.0),
        'c_sample': nrm(ks[6], (DEC_BATCH, D_MODEL), 1.0),
        'w_ada': nrm(ks[7], (DEPTH, D_MODEL, 6 * D_MODEL), 0.3 * D_MODEL ** -0.5),
        'b_ada': nrm(ks[8], (DEPTH, 6 * D_MODEL), 0.02),
        'norm1_g': 1.0 + nrm(ks[9], (DEPTH, D_MODEL), 0.02),
        'norm2_g': 1.0 + nrm(ks[10], (DEPTH, D_MODEL), 0.02),
        'w_in': nrm(ks[11], (DEPTH, D_MODEL, IN_WIDTH), D_MODEL ** -0.5),
        'w_alpha2': nrm(ks[12], (DEPTH, GATE_RANK, GLA_DK), GATE_RANK ** -0.5),
        'b_alpha': nrm(ks[13], (DEPTH, GLA_DK), 0.1),
        'gla_norm_g': 1.0 + nrm(ks[14], (DEPTH, GLA_HEAD_V), 0.02),
        'swa_sinks': nrm(ks[15], (DEPTH, SWA_HQ), 1.0),
        'w_out': nrm(ks[16], (DEPTH, D_MODEL, D_MODEL), D_MODEL ** -0.5),
        'peer_wq': nrm(ks[17], (DEPTH, D_MODEL, PEER_HEADS * PEER_QDIM), D_MODEL ** -0.5),
        'peer_keys': nrm(ks[18], (DEPTH, PEER_HEADS, 2, N_KEYS, PEER_HALF), PEER_HALF ** -0.5),
        'peer_u': nrm(ks[19], (DEPTH, N_EXPERTS, D_MODEL), D_MODEL ** -0.5),
        'peer_v': nrm(ks[20], (DEPTH, N_EXPERTS, D_MODEL), 1.0),
        'final_g': 1.0 + nrm(ks[21], (D_MODEL,), 0.02),
    }


def reference(x_prompt, x_sample, state_gla, cache_swa_k, cache_swa_v, c_prompt, c_sample, w_ada, b_ada,
              norm1_g, norm2_g, w_in, w_alpha2, b_alpha, gla_norm_g, swa_sinks, w_out, peer_wq, peer_keys,
              peer_u, peer_v, final_g):
    xp, xs = x_prompt, x_sample
    gla_p, kp, vp, gla_s, ksm, vsm = [], [], [], [], [], []
    for l in range(DEPTH):
        w = (w_ada[l], b_ada[l], norm1_g[l], norm2_g[l], w_in[l], w_alpha2[l], b_alpha[l], gla_norm_g[l],
             swa_sinks[l], w_out[l], peer_wq[l], peer_keys[l], peer_u[l], peer_v[l])
        xp, s_p, k_p, v_p = decoder_layer(xp, c_prompt, None, None, None, *w)
        xs, s_s, k_s, v_s = decoder_layer(xs, c_sample, state_gla[l], cache_swa_k[l], cache_swa_v[l], *w)
        gla_p.append(s_p); kp.append(k_p); vp.append(v_p)
        gla_s.append(s_s); ksm.append(k_s); vsm.append(v_s)
    y_prompt = rmsnorm(xp, final_g)
    y_sample = rmsnorm(xs, final_g)
    return (y_prompt, y_sample, jnp.stack(gla_p), jnp.stack(kp), jnp.stack(vp),
            jnp.stack(gla_s), jnp.stack(ksm), jnp.stack(vsm))
```

```python
import numpy as np
import concourse.bass as bass
import concourse.mybir as mybir
from concourse.bass_utils import run_bass_kernel_spmd

F32 = mybir.dt.float32
BF16 = mybir.dt.bfloat16
U8 = mybir.dt.uint8
AF = mybir.ActivationFunctionType
ALU = mybir.AluOpType

NCORES = 8
D = 2048
DEPTH = 2
SEQ = 2048
NSEQ_S = 16
LS = 4
TS = NSEQ_S * LS
TTOK = SEQ + TS
INW = 12816
OFF_GQ, OFF_GK, OFF_GV, OFF_GR, OFF_GLR, OFF_SQ, OFF_SK, OFF_SV, OFF_GA, OFF_GB = (
    0, 1024, 2048, 4096, 6144, 6160, 8208, 8464, 8720, 10768)
EPS = 1e-6
EPOCH = 24000
NEG = -1.0e30
import os as _os
FLAGS = set(_os.environ.get("MKFLAGS", "").split(","))


class V:
    __slots__ = ("tile", "ap")

    def __init__(self, tile, ap):
        self.tile = tile
        self.ap = ap

    def __getitem__(self, idx):
        return V(self.tile, self.ap[idx])

    def bc(self, shape):
        return V(self.tile, self.ap.to_broadcast(list(shape)))

    def re(self, s, **kw):
        return V(self.tile, self.ap.rearrange(s, **kw))

    def unsq(self, ax):
        return V(self.tile, self.ap.unsqueeze(ax))

    @property
    def shape(self):
        return tuple(self.ap.shape)


class T:
    def __init__(self, t, name, space, rng=None):
        self.t = t
        self.name = name
        self.space = space
        self.last_w = None
        self.reads = []
        self.dsem = None
        self.dval = 0
        self.ro = False
        self.wo = False
        self.rng = rng
        self.aliases = []

    def __getitem__(self, idx):
        return V(self, self.t[idx])

    @property
    def v(self):
        return V(self, self.t)


class EngS:
    def __init__(self, name):
        self.name = name
        self.prog = []
        self.sems = []
        self.cnt = 0
        self.seen = {}


class K:
    def __init__(self, nc):
        self.nc = nc
        self.E = {n: EngS(n) for n in ("tensor", "vector", "scalar", "gpsimd", "sync")}
        self.nsem = 0
        self.ntile = 0
        self.dma_tiles = []
        self.swq = []
        self.arena = None
        self.carved = []

    def _sem(self, nm):
        self.nsem += 1
        return self.nc.alloc_semaphore(f"s{self.nsem}_{nm}"[:40])

    def make_arena(self, nbytes):
        self.arena = self.nc.alloc_sbuf_tensor("arena", [128, nbytes], U8)
        self.arena_bytes = nbytes

    def carve(self, off, shape, dt, name):
        isz = 4 if dt == F32 else (2 if dt == BF16 else 1)
        n = 1
        for s in shape[1:]:
            n *= s
        nb = n * isz
        assert off % 4 == 0 and off + nb <= self.arena_bytes, (name, off, nb, self.arena_bytes)
        ap = self.arena[0:shape[0], off:off + nb].bitcast(dt)
        if len(shape) == 3:
            ap = ap.rearrange("p (a b) -> p a b", a=shape[1])
        elif len(shape) == 4:
            ap = ap.rearrange("p (a b c) -> p a b c", a=shape[1], b=shape[2])
        self.ntile += 1
        t = T(ap, f"{name}_{self.ntile}", "sb", (off, off + nb))
        for o in self.carved:
            if o.rng[0] < t.rng[1] and t.rng[0] < o.rng[1]:
                o.aliases.append(t)
                t.aliases.append(o)
        self.carved.append(t)
        return t

    def ps(self, name):
        self.ntile += 1
        h = self.nc.alloc_psum_tensor(f"{name}_{self.ntile}", [128, 512], F32)
        return T(h[:, :], name, "ps")

    def dram(self, name, shape, dt, kind):
        h = self.nc.dram_tensor(name, list(shape), dt, kind=kind)
        t = T(h.ap(), name, "dram")
        if kind == "ExternalInput":
            t.ro = True
        if kind == "ExternalOutput":
            t.wo = True
        return t

    def _need(self, E, sem, val):
        key = id(sem)
        if val <= 0 or E.seen.get(key, 0) >= val:
            return
        E.seen[key] = val
        E.prog.append(("wait", sem, val))

    def _deps(self, E, reads, writes):
        for t0 in reads:
            if t0.ro:
                continue
            for t in [t0] + t0.aliases:
                if t.last_w is not None:
                    sem, val, en = t.last_w
                    if not (en == E.name and en == "tensor"):
                        self._need(E, sem, val)
                if t.space == "ps":
                    for (sem, val, en) in t.reads:
                        if en != E.name:
                            self._need(E, sem, val)
        for t0 in writes:
            for t in [t0] + t0.aliases:
                if t.last_w is not None:
                    sem, val, en = t.last_w
                    if en != E.name:
                        self._need(E, sem, val)
                for (sem, val, en) in t.reads:
                    if en != E.name:
                        self._need(E, sem, val)

    def op(self, eng, fn, reads=(), writes=()):
        E = self.E[eng]
        self._deps(E, reads, writes)
        if not E.sems or E.cnt >= EPOCH:
            E.sems.append(self._sem(eng))
            E.cnt = 0
        E.cnt += 1
        sem = E.sems[-1]
        E.prog.append(("op", fn, sem))
        rec = (sem, E.cnt, eng)
        for t in reads:
            if t.ro:
                continue
            t.reads.append(rec)
            if len(t.reads) > 48:
                d = {}
                for r in t.reads:
                    kk = id(r[0])
                    if kk not in d or d[kk][1] < r[1]:
                        d[kk] = r
                t.reads = list(d.values())
        for t in writes:
            t.last_w = rec
            t.reads = []

    def dma(self, eng, out, in_):
        E = self.E[eng]
        out_t, in_t = out.tile, in_.tile
        if eng == "gpsimd":
            def ndesc(ap):
                n = 1
                for d in ap.shape[:-1]:
                    n *= d
                return n
            nd = (max(ndesc(out.ap), ndesc(in_.ap)) + 15) // 16 + 2
            fifo = self.swq
            while fifo and sum(x[2] for x in fifo) + nd > 640:
                sem0, val0, _ = fifo.pop(0)
                self._need(E, sem0, val0)
        self._deps(E, [in_t], [out_t])
        sb_t = out_t if out_t.space != "dram" else in_t
        if sb_t.dsem is None:
            sb_t.dsem = self._sem("d" + sb_t.name)
            self.dma_tiles.append(sb_t)
        sb_t.dval += 16
        sem, val = sb_t.dsem, sb_t.dval
        E.prog.append(("dma", out.ap, in_.ap, sem))
        if eng == "gpsimd":
            self.swq.append((sem, val, nd))
        if not out_t.ro and not out_t.wo:
            out_t.last_w = (sem, val, "dma")
            out_t.reads = []
        if not in_t.ro:
            in_t.reads.append((sem, val, "dma"))

    def emit(self):
        nc = self.nc
        S = self.E["sync"]
        for t in self.dma_tiles:
            self._need(S, t.dsem, t.dval)
        with nc.Block() as block:
            def run(E, eng):
                for ent in E.prog:
                    if ent[0] == "wait":
                        eng.wait_ge(ent[1], ent[2])
                    elif ent[0] == "op":
                        ent[1](eng).then_inc(ent[2], 1)
                    else:
                        eng.dma_start(out=ent[1], in_=ent[2]).then_inc(ent[3], 16)

            @block.tensor
            def _(e):
                run(self.E["tensor"], e)

            @block.vector
            def _(e):
                run(self.E["vector"], e)

            @block.scalar
            def _(e):
                run(self.E["scalar"], e)

            @block.gpsimd
            def _(e):
                run(self.E["gpsimd"], e)

            @block.sync
            def _(e):
                run(self.E["sync"], e)

    def stats(self):
        return ({n: sum(1 for x in e.prog if x[0] != "wait") for n, e in self.E.items()},
                {n: sum(1 for x in e.prog if x[0] == "wait") for n, e in self.E.items()}, self.nsem)


CF = {}
_o = 0
for _n, _w in [("ident", 128), ("ones", 128), ("trip", 128), ("dp", 128), ("mtp", 128),
               ("tris", 64), ("ds", 64), ("mts", 64), ("ssels", 16), ("ssel01", 16), ("sselp", 1),
               ("blk01s", 64)]:
    CF[_n] = (_o, _w)
    _o += _w
NCF = _o
CB = {}
_o = 0
for _n, _w in [("ones", 128), ("swam", 256), ("sel24", 8), ("pastm", 32), ("newm", 512)]:
    CB[_n] = (_o, _w)
    _o += _w
NCB = _o


def make_consts():
    cf = np.zeros((128, NCF), np.float32)
    cb = np.zeros((128, NCB), np.float32)

    def put(dst, tab, name, arr):
        o, w = tab[name]
        dst[:arr.shape[0], o:o + w] = arr

    i = np.arange(128)
    put(cf, CF, "ident", np.eye(128, dtype=np.float32))
    put(cf, CF, "ones", np.ones((128, 128), np.float32))
    le = (i[:, None] <= i[None, :]).astype(np.float32)
    gt = (i[:, None] > i[None, :]).astype(np.float32)
    put(cf, CF, "trip", le * (-1.0 / 16.0))
    put(cf, CF, "dp", gt * (-1.0 / 16.0))
    put(cf, CF, "mtp", le)
    put(cf, CF, "sselp", np.full((128, 1), -1.0 / 16.0, np.float32))
    j = np.arange(64)
    same = (j[:, None] // 4 == j[None, :] // 4)
    les = (same & (j[:, None] <= j[None, :])).astype(np.float32)
    gts = (same & (j[:, None] > j[None, :])).astype(np.float32)
    put(cf, CF, "tris", les * (-1.0 / 16.0))
    put(cf, CF, "ds", gts * (-1.0 / 16.0))
    put(cf, CF, "mts", les)
    sel = (j[:, None] // 4 == np.arange(16)[None, :]).astype(np.float32)
    put(cf, CF, "ssels", sel * (-1.0 / 16.0))
    put(cf, CF, "ssel01", sel)
    put(cf, CF, "blk01s", same.astype(np.float32))
    put(cb, CB, "ones", np.ones((128, 128), np.float32))
    put(cb, CB, "swam", np.concatenate([gt, le], axis=1))
    s24 = np.zeros((24, 8), np.float32)
    for h in range(8):
        s24[3 * h:3 * h + 3, h] = 1.0
    put(cb, CB, "sel24", s24)
    pm = (i[:, None] > (np.arange(32)[None, :] % 4)).astype(np.float32)
    put(cb, CB, "pastm", pm)
    nm = np.zeros((64, 16, 32), np.float32)
    for s_ in range(64):
        for c in range(32):
            if (s_ % 4) <= (c % 4):
                nm[s_, s_ // 4, c] = 1.0
    put(cb, CB, "newm", nm.reshape(64, 512))
    return cf, cb


NPP = 16 + 16 + 96 + 4 + 16
PP_G1, PP_G2, PP_BADA, PP_GNG, PP_SINK = 0, 16, 32, 128, 132


class Prog:
    def __init__(self, dbg=()):
        self.dbg = set(dbg)
        self.dbg_out = {}
        nc = bass.Bass("TRN2", target_bir_lowering=False)
        self.nc = nc
        k = K(nc)
        self.k = k
        dr = k.dram
        self.xT_in = dr("xT", [D, TTOK], F32, "ExternalInput")
        self.cT_in = dr("cT", [D, 1 + TS], F32, "ExternalInput")
        self.state_in = dr("state", [DEPTH, NSEQ_S, 4, 256, 512], F32, "ExternalInput")
        self.ckT_in = dr("ckT", [DEPTH, 64, 4, 2048 + 64], F32, "ExternalInput")
        self.ck_in = dr("ck", [DEPTH, NSEQ_S, 128, 256], F32, "ExternalInput")
        self.cv_in = dr("cv", [DEPTH, NSEQ_S, 128, 256], F32, "ExternalInput")
        self.w_ada = dr("w_ada", [DEPTH, D, 6 * D], F32, "ExternalInput")
        self.w_in = dr("w_in", [DEPTH, D, INW], F32, "ExternalInput")
        self.w_out = dr("w_out", [DEPTH, D, D], F32, "ExternalInput")
        self.peer_wq = dr("peer_wq", [DEPTH, D, D], F32, "ExternalInput")
        self.uT = dr("uT", [DEPTH, D, 16384], F32, "ExternalInput")
        self.pv = dr("pv", [DEPTH, 16384, D], F32, "ExternalInput")
        self.pkT_in = dr("pkT", [DEPTH, 128, 16, 128], F32, "ExternalInput")
        self.pp_in = dr("pp", [DEPTH, 128, NPP], F32, "ExternalInput")
        self.fg_in = dr("fg", [128, 16], F32, "ExternalInput")
        self.wal_in = dr("wal1", [DEPTH, 17, 1024], F32, "ExternalInput")
        self.cf_in = dr("cf", [128, NCF], F32, "ExternalInput")
        self.cb_in = dr("cb", [128, NCB], F32, "ExternalInput")
        self.yT_out = dr("yT", [D, TTOK], F32, "ExternalOutput")
        self.glap_out = dr("gla_p", [DEPTH, 4, 256, 512], F32, "ExternalOutput")
        self.kp_out = dr("k_p", [DEPTH, 128, 256], F32, "ExternalOutput")
        self.vp_out = dr("v_p", [DEPTH, 128, 256], F32, "ExternalOutput")
        self.glas_out = dr("gla_s", [DEPTH, NSEQ_S, 4, 256, 512], F32, "ExternalOutput")
        self.ks_out = dr("k_s", [DEPTH, NSEQ_S, 128, 256], F32, "ExternalOutput")
        self.vs_out = dr("v_s", [DEPTH, NSEQ_S, 128, 256], F32, "ExternalOutput")
        self.mods = dr("mods", [DEPTH, 128, 96, TS], F32, "Internal")

        self.banks = [k.ps(f"bank{i}") for i in range(8)]
        self.bank_i = 0
        self.slot_i = 0
        self.alloc()
        self.build()
        k.emit()

    def bank(self):
        b = self.banks[self.bank_i % 8]
        self.bank_i += 1
        return b

    def mm(self, out, lhsT, rhs, start=True, stop=True):
        self.k.op("tensor", lambda e: e.matmul(out.ap, lhsT=lhsT.ap, rhs=rhs.ap, start=start, stop=stop),
                  [lhsT.tile, rhs.tile], [out.tile])

    def transpose(self, out, in_, ident):
        self.k.op("tensor", lambda e: e.transpose(out.ap, in_.ap, ident.ap), [in_.tile, ident.tile], [out.tile])

    def act(self, out, in_, func, scale=None, bias=None, alpha=None, accum=None):
        kw = {}
        reads = [in_.tile]
        writes = [out.tile]
        if scale is not None:
            if isinstance(scale, V):
                kw["scale"] = scale.ap
                reads.append(scale.tile)
            else:
                kw["scale"] = float(scale)
        if bias is not None:
            if isinstance(bias, V):
                kw["bias"] = bias.ap
                reads.append(bias.tile)
            else:
                kw["bias"] = float(bias)
        if alpha is not None:
            kw["alpha"] = float(alpha)
        if accum is not None:
            kw["accum_out"] = accum.ap
            writes.append(accum.tile)
        self.k.op("scalar", lambda e: e.activation(out=out.ap, in_=in_.ap, func=func, **kw), reads, writes)

    def tt(self, out, a, b, op, eng="vector"):
        self.k.op(eng, lambda e: e.tensor_tensor(out=out.ap, in0=a.ap, in1=b.ap, op=op),
                  [a.tile, b.tile], [out.tile])

    def ts(self, out, a, s1, op0, s2=None, op1=None, eng="vector"):
        reads = [a.tile]
        a1 = s1
        a2 = s2
        if isinstance(s1, V):
            reads.append(s1.tile)
            a1 = s1.ap
        if isinstance(s2, V):
            reads.append(s2.tile)
            a2 = s2.ap
        if op1 is None:
            self.k.op(eng, lambda e: e.tensor_scalar(out=out.ap, in0=a.ap, scalar1=a1, scalar2=None, op0=op0),
                      reads, [out.tile])
        else:
            self.k.op(eng, lambda e: e.tensor_scalar(out=out.ap, in0=a.ap, scalar1=a1, scalar2=a2, op0=op0, op1=op1),
                      reads, [out.tile])

    def stt(self, out, a, s, b, op0, op1):
        reads = [a.tile, b.tile]
        sa = s
        if isinstance(s, V):
            reads.append(s.tile)
            sa = s.ap
        self.k.op("vector", lambda e: e.scalar_tensor_tensor(out=out.ap, in0=a.ap, scalar=sa, in1=b.ap, op0=op0, op1=op1),
                  reads, [out.tile])

    def cp(self, out, in_, eng="vector"):
        if eng == "scalar":
            self.act(out, in_, AF.Copy)
        else:
            self.k.op(eng, lambda e: e.tensor_copy(out=out.ap, in_=in_.ap), [in_.tile], [out.tile])

    def memset(self, out, val, eng="vector"):
        self.k.op(eng, lambda e: e.memset(out.ap, val), [], [out.tile])

    def recip(self, out, in_):
        self.k.op("vector", lambda e: e.reciprocal(out=out.ap, in_=in_.ap), [in_.tile], [out.tile])

    def max8(self, out, in_):
        self.k.op("vector", lambda e: e.max(out=out.ap, in_=in_.ap), [in_.tile], [out.tile])

    def mrep(self, out, rep, vals):
        self.k.op("vector", lambda e: e.match_replace(out=out.ap, in_to_replace=rep.ap, in_values=vals.ap, imm_value=NEG),
                  [rep.tile, vals.tile], [out.tile])

    def dma(self, out, in_, eng="sync"):
        self.k.dma(eng, out, in_)

    def dump(self, name, v, dt=F32):
        if name not in self.dbg:
            return
        shp = list(v.shape)
        o = self.k.dram("dbg_" + name, shp, dt, "ExternalOutput")
        self.dbg_out[name] = o
        self.dma(o.v, v, eng="gpsimd" if dt != v.ap.dtype else "sync")

    def alloc(self):
        k = self.k
        AB = 206 * 1024
        k.make_arena(AB)
        o = 0

        def take(nb):
            nonlocal o
            r = o
            o += (nb + 31) // 32 * 32
            return r

        c = k.carve
        self.cf = c(take(NCF * 4), [128, NCF], F32, "cf")
        self.cb = c(take(NCB * 2), [128, NCB], BF16, "cb")
        self.S = [c(take(16384), [128, 8, 512], F32, f"S{l}") for l in range(DEPTH)]
        self.pskT = [c(take(1024), [128, 4, 128], BF16, f"pskT{l}") for l in range(DEPTH)]
        self.psv = [c(take(512), [128, 256], BF16, f"psv{l}") for l in range(DEPTH)]
        self.pp = [c(take(NPP * 4), [128, NPP], F32, f"pp{l}") for l in range(DEPTH)]
        self.fg = c(take(64), [128, 16], F32, "fg")
        self.modp = [c(take(96 * 4), [128, 96], F32, f"modp{l}") for l in range(DEPTH)]
        self.A1 = [c(take(64), [128, 16], F32, f"A1{l}") for l in range(DEPTH)]
        self.A2 = [c(take(64), [128, 16], F32, f"A2{l}") for l in range(DEPTH)]
        self.esink = [c(take(64), [128, 16], F32, f"esink{l}") for l in range(DEPTH)]
        self.smallf = c(take(1024), [128, 256], F32, "smallf")
        self.slots = [c(take(16384), [128, 16, 512], BF16, f"slot{i}") for i in range(2)]
        self.base = o
        self.layouts = {512: self.layout(512), 64: self.layout(64)}
        self.newm = self.cbv("newm", 64).re("p (b c) -> p b c", b=16)

    def layout(self, TT):
        k = self.k
        c = k.carve
        o = self.base
        NST = 1 if TT == 64 else 4
        TP = min(TT, 128)
        B = {}

        def take(nb):
            nonlocal o
            r = o
            o += (nb + 31) // 32 * 32
            return r

        B["xT"] = c(take(16 * TT * 4), [128, 16, TT], F32, "xT")
        B["hT"] = c(take(16 * TT * 2), [128, 16, TT], BF16, "hT")
        ph = o
        B["sq"] = [c(take(TT * 4), [128, TT], F32, "sq") for _ in range(2)]
        B["rstd"] = c(take(TT * 4), [128, TT], F32, "rstd")
        B["ntmp"] = [c(take(TT * 4), [128, TT], F32, "ntmp") for _ in range(2)]
        o = ph
        B["qT"] = c(take(8 * TT * 2), [128, 8, TT], BF16, "qT")
        B["kT"] = c(take(8 * TT * 2), [128, 8, TT], BF16, "kT")
        B["ktok"] = c(take(NST * 1024 * 2), [TP, NST, 1024], BF16, "ktok")
        vt_off = take(NST * 2048 * 2)
        B["vtok"] = c(vt_off, [TP, NST, 2048], BF16, "vtok")
        B["onT"] = c(take(16 * TT * 2), [128, 16, TT], BF16, "onT")
        B["glrT1"] = c(take(TT * 2), [17, TT], BF16, "glrT1")
        B["wal1"] = c(take(1024 * 2), [17, 1024], BF16, "wal1")
        tr = o
        B["l"] = c(take(4096), [TP, 1024], F32, "l")
        B["ebe"] = B["l"]
        B["eb"] = c(take(8 * TP * 4), [128, 8, TP], F32, "eb")
        B["enb"] = c(take(8 * TP * 4), [128, 8, TP], F32, "enb")
        B["khat"] = c(take(2048), [TP, 1024], BF16, "khat")
        B["attT"] = c(take(4 * TP * 2), [TP, 4, TP], BF16, "attT")
        B["osq"] = c(take(4 * TP * 4), [128, 4, TP], F32, "osq")
        B["orstd"] = c(take(TP * 4), [128, TP], F32, "orstd")
        B["ebend"] = c(take(8 * 16 * 4), [128, 8, 16], F32, "ebend")
        if TT == 64:
            B["S0"] = [c(take(4096), [128, 2, 512], F32, "S0") for _ in range(2)]
            B["Snew"] = [c(take(2048), [128, 512], F32, "Snew") for _ in range(2)]
            B["khm"] = [c(take(2048), [TP, 1024], BF16, "khm") for _ in range(2)]
            B["ckT"] = c(take(64 * 128 * 2), [128, 64, 128], BF16, "ckT")
            B["cv"] = c(take(16 * 256 * 2), [128, 16, 256], BF16, "cv")
            B["knew"] = c(take(512 * 4), [TP, 512], F32, "knew")
            B["mA"] = c(take(16 * TT * 4), [128, 16, TT], F32, "mA")
            B["mS"] = c(take(16 * TT * 4), [128, 16, TT], F32, "mS")
            B["mG"] = c(take(16 * TT * 4), [128, 16, TT], F32, "mG")
            B["PTs"] = [c(take(64), [128, 32], BF16, "PTs") for _ in range(2)]
            B["PTn"] = [c(take(64), [TP, 32], BF16, "PTn") for _ in range(2)]
            B["rtmp"] = [c(take(TT * 4), [128, TT], F32, "rtmp") for _ in range(2)]
            tr = o
        end_mixer = o
        o = ph
        B["sqT"] = c(take(16 * TT * 2), [128, 16, TT], BF16, "sqT")
        o = vt_off
        B["oswaT"] = c(take(16 * TT * 2), [128, 16, TT], BF16, "oswaT")
        o = tr
        B["skT"] = c(take(4 * (128 + TT) * 2), [128, 4, 128 + TT], BF16, "skT")
        B["sv1"] = c(take((NST + 1) * 256 * 2), [TP, NST + 1, 256], BF16, "sv1")
        B["PT"] = [c(take(512), [128, 256], BF16, "PT") for _ in range(2)]
        B["kvo"] = c(take(512 * 4), [TP, 512], F32, "kvo")
        B["rden"] = [c(take(512), [128, 128], F32, "rden") for _ in range(2)]
        B["gtmp"] = [c(take(TT * 4), [128, TT], F32, "gtmp") for _ in range(2)]
        end_mixer = max(end_mixer, o)
        o = ph
        B["pqT"] = c(take(16 * TT * 2), [128, 16, TT], BF16, "pqT")
        B["gbc"] = c(take(8 * TT * 4), [128, 8, TT], F32, "gbc")
        B["pkT"] = c(take(16 * 128 * 2), [128, 16, 128], BF16, "pkT")
        B["thT"] = c(take(TT * 2), [24, TT], BF16, "thT")
        B["gfT"] = c(take(TT * 4), [8, TT], F32, "gfT")
        pk = o
        B["sc"] = c(take(16 * 128 * 4), [TP, 16, 128], F32, "sc")
        B["scr"] = c(take(512), [TP, 128], F32, "scr")
        B["top16"] = c(take(16 * 16 * 4), [TP, 16, 16], F32, "top16")
        B["comb"] = [c(take(1024), [TP, 256], F32, "comb") for _ in range(3)]
        B["c24"] = c(take(8 * 24 * 4), [TP, 8, 24], F32, "c24")
        B["pm"] = c(take(256 * 4), [TP, 256], F32, "pm")
        B["pmb"] = c(take(64 * 2), [TP, 64], BF16, "pmb")
        o = pk
        B["hraw"] = c(take(4 * TT * 4), [128, 4, TT], F32, "hraw")
        B["zc"] = [c(take(TT * 4), [128, TT], F32, "zc") for _ in range(2)]
        B["E"] = [c(take(TT * 4), [128, TT], F32, "E") for _ in range(2)]
        B["ptmp"] = [c(take(TT * 4), [128, TT], F32, "ptmp") for _ in range(2)]
        B["acc"] = [c(take(TT * 4), [128, TT], F32, "acc") for _ in range(2)]
        B["AT"] = [c(take(4 * TT * 2), [128, 4, TT], BF16, "AT") for _ in range(2)]
        end_peer = o
        B["_end"] = max(end_mixer, end_peer)
        return B

    def slot(self):
        s = self.slots[self.slot_i % 2]
        self.slot_i += 1
        return s

    def wload(self, s, W, l, c0, n, dst=0):
        src = V(W, W.t[l].rearrange("(kc p) n -> p kc n", p=128)[:, :, c0:c0 + n])
        self.dma(s[:, :, dst:dst + n], src, eng="gpsimd")

    def proj_fm(self, s, ncols, hT, TT, consume, col0=0):
        nch = (ncols + 127) // 128
        for cc in range(nch):
            M = min(128, ncols - cc * 128)
            ps = self.bank()
            for kc in range(16):
                self.mm(ps[0:M, 0:TT], s[:, kc, col0 + cc * 128:col0 + cc * 128 + M], hT[:, kc, 0:TT],
                        start=(kc == 0), stop=(kc == 15))
            consume(cc, ps[0:M, 0:TT])

    def proj_tm(self, s, ncols, hT, subt, consume):
        for si, (t0, n) in enumerate(subt):
            ps = self.bank()
            for kc in range(16):
                self.mm(ps[0:n, 0:ncols], hT[:, kc, t0:t0 + n], s[:, kc, 0:ncols], start=(kc == 0), stop=(kc == 15))
            consume(si, ps[0:n, 0:ncols])

    def cfv(self, name, rows=128):
        o, w = CF[name]
        return self.cf[0:rows, o:o + w]

    def cbv(self, name, rows=128):
        o, w = CB[name]
        return self.cb[0:rows, o:o + w]

    def build(self):
        self.dma(self.cf.v, self.cf_in.v)
        self.dma(self.cb.v, self.cb_in.v, eng="gpsimd")
        self.dma(self.fg.v, self.fg_in.v)
        for l in range(DEPTH):
            self.dma(self.pp[l].v, self.pp_in[l])
            self.act(self.esink[l].v, self.pp[l][:, PP_SINK:PP_SINK + 16], AF.Exp)
        self.memset(self.smallf[:, 0:1], EPS)
        self.memset(self.smallf[:, 1:2], 1.0)
        self.compute_mod()
        for l in range(DEPTH):
            self.dump(f"modp{l}", self.modp[l].v)
        tiles = [("s", SEQ, TS)] + [("p", i * 512, 512) for i in range(4)]
        if "tiles_s" in FLAGS:
            tiles = tiles[:1]
        if "tiles_p0" in FLAGS:
            tiles = tiles[1:2]
        if "tiles_sp0" in FLAGS:
            tiles = tiles[0:2]
        for (kind, t0, TT) in tiles:
            self.run_tile(kind, t0, TT)

    def compute_mod(self):
        B = self.layouts[512]
        NCOL = 1 + TS
        cs = B["xT"]
        sc = B["hT"]
        stage = B["onT"]
        stg = B["gbc"]
        self.dma(cs[:, :, 0:NCOL], V(self.cT_in, self.cT_in.t.rearrange("(kc p) n -> p kc n", p=128)))
        self.act(sc[:, :, 0:NCOL], cs[:, :, 0:NCOL], AF.Silu)
        for l in range(DEPTH):
            for g in range(24):
                s = self.slot()
                self.wload(s, self.w_ada, l, g * 512, 512)

                def consume(cc, ps, g=g, l=l):
                    ch = g * 4 + cc
                    self.act(self.modp[l][:, ch:ch + 1], ps[:, 0:1], AF.Identity,
                             bias=self.pp[l][:, PP_BADA + ch:PP_BADA + ch + 1])
                    self.act(stg[:, ch % 8, 0:TS], ps[:, 1:NCOL], AF.Identity,
                             bias=self.pp[l][:, PP_BADA + ch:PP_BADA + ch + 1])
                    if ch % 8 == 7:
                        c8 = ch - 7
                        self.dma(self.mods[l][:, c8:c8 + 8, :], stg[:, :, 0:TS])
                self.proj_fm(s, 512, sc, NCOL, consume)
            self.stt(self.A1[l].v, self.modp[l][:, 16:32], 1.0, self.pp[l][:, PP_G1:PP_G1 + 16], ALU.add, ALU.mult)
            self.stt(self.A2[l].v, self.modp[l][:, 64:80], 1.0, self.pp[l][:, PP_G2:PP_G2 + 16], ALU.add, ALU.mult)

    def run_tile(self, kind, t0, TT):
        B = self.layouts[TT]
        xsrc = V(self.xT_in, self.xT_in.t.rearrange("(c p) t -> p c t", p=128)[:, :, t0:t0 + TT])
        self.dma(B["xT"].v, xsrc)
        for l in range(1 if "depth1" in FLAGS else DEPTH):
            self.layer(kind, t0, TT, l, B)
        self.final_norm(kind, t0, TT, B)

    def rms_rstd(self, B, TT):
        xT = B["xT"]
        ps = self.bank()
        for c in range(16):
            sq = B["sq"][c % 2]
            self.act(sq.v, xT[:, c, :], AF.Square)
            self.mm(ps[:, 0:TT], self.cfv("ones"), sq.v, start=(c == 0), stop=(c == 15))
        self.act(B["rstd"].v, ps[:, 0:TT], AF.Ln, scale=1.0 / D, bias=self.smallf[:, 0:1])
        self.act(B["rstd"].v, B["rstd"].v, AF.Exp, scale=-0.5)

    def load_mod_s(self, B, l, which):
        return V(self.mods, self.mods.t[l][:, which * 16:(which + 1) * 16, :])

    def norm_mod(self, kind, B, TT, l, which):
        self.rms_rstd(B, TT)
        xT, hT = B["xT"], B["hT"]
        if kind == "p":
            A = (self.A1 if which == 1 else self.A2)[l]
            sh0 = 0 if which == 1 else 48
            for c in range(16):
                tmp = B["ntmp"][c % 2]
                self.stt(tmp.v, xT[:, c, :], A[:, c:c + 1], B["rstd"].v, ALU.mult, ALU.mult)
                self.act(hT[:, c, :], tmp.v, AF.Identity, bias=self.modp[l][:, sh0 + c:sh0 + c + 1])
        else:
            g0 = PP_G1 if which == 1 else PP_G2
            sc_i, sh_i = (1, 0) if which == 1 else (4, 3)
            self.dma(B["mA"].v, self.load_mod_s(B, l, sc_i))
            self.dma(B["mS"].v, self.load_mod_s(B, l, sh_i))
            for c in range(16):
                self.ts(B["mA"][:, c, :], B["mA"][:, c, :], 1.0, ALU.add,
                        self.pp[l][:, g0 + c:g0 + c + 1], ALU.mult)
                tmp = B["ntmp"][c % 2]
                self.tt(tmp.v, xT[:, c, :], B["rstd"].v, ALU.mult)
                self.tt(tmp.v, tmp.v, B["mA"][:, c, :], ALU.mult)
                self.tt(hT[:, c, :], tmp.v, B["mS"][:, c, :], ALU.add)

    def residual(self, kind, B, l, which, c, ps):
        xT = B["xT"]
        if kind == "p":
            g0 = 32 if which == 1 else 80
            self.stt(xT[:, c, :], ps, self.modp[l][:, g0 + c:g0 + c + 1], xT[:, c, :], ALU.mult, ALU.add)
        else:
            tmp = B["rtmp"][c % 2]
            self.tt(tmp.v, ps, B["mG"][:, c, :], ALU.mult)
            self.tt(xT[:, c, :], xT[:, c, :], tmp.v, ALU.add)

    def final_norm(self, kind, t0, TT, B):
        self.rms_rstd(B, TT)
        ydst = self.yT_out.t.rearrange("(c p) t -> p c t", p=128)
        for c in range(16):
            tmp = B["ntmp"][c % 2]
            self.stt(tmp.v, B["xT"][:, c, :], self.fg[:, c:c + 1], B["rstd"].v, ALU.mult, ALU.mult)
            self.dma(V(self.yT_out, ydst[:, c, t0:t0 + TT]), tmp.v)

    def bank_ex(self, exclude):
        while True:
            b = self.banks[self.bank_i % 8]
            self.bank_i += 1
            if (self.bank_i - 1) % 8 not in exclude:
                return b

    def layer(self, kind, t0, TT, l, B):
        if "nomixer" not in FLAGS:
            self.mixer(kind, t0, TT, l, B)
        self.dump(f"x1{kind}{l}_{t0}", B["xT"].v)
        if "nopeer" not in FLAGS:
            self.peer(kind, t0, TT, l, B)
        self.dump(f"x2{kind}{l}_{t0}", B["xT"].v)

    def mixer(self, kind, t0, TT, l, B):
        P = kind == "p"
        subt = [(i * 128, 128) for i in range(4)] if P else [(0, TS)]
        hT = B["hT"]
        self.norm_mod(kind, B, TT, l, 1)
        self.dump(f"h{kind}{l}_{t0}", hT.v, BF16)
        self.dma(B["wal1"].v, self.wal_in[l], eng="gpsimd")
        self.memset(B["glrT1"].v, 1.0)
        s = self.slot()
        self.wload(s, self.w_in, l, OFF_GLR, 16)
        self.proj_fm(s, 16, hT, TT, lambda cc, ps: self.cp(B["glrT1"][0:16, :], ps, eng="scalar"))
        for g in range(2):
            s = self.slot()
            self.wload(s, self.w_in, l, OFF_GQ + g * 512, 512)
            self.proj_fm(s, 512, hT, TT, lambda cc, ps, g=g: self.cp(B["qT"][:, g * 4 + cc, :], ps, eng="scalar"))
        for g in range(2):
            s = self.slot()
            self.wload(s, self.w_in, l, OFF_GK + g * 512, 512)
            self.proj_fm(s, 512, hT, TT, lambda cc, ps, g=g: self.cp(B["kT"][:, g * 4 + cc, :], ps, eng="vector"))
            self.proj_tm(s, 512, hT, subt,
                         lambda si, ps, g=g: self.cp(B["ktok"][:, si, g * 512:(g + 1) * 512], ps, eng="scalar"))
        for g in range(4):
            s = self.slot()
            self.wload(s, self.w_in, l, OFF_GV + g * 512, 512)
            self.proj_tm(s, 512, hT, subt,
                         lambda si, ps, g=g: self.cp(B["vtok"][:, si, g * 512:(g + 1) * 512], ps,
                                                     eng="vector" if si % 2 else "scalar"))
        for ci, (c0, C) in enumerate(subt):
            if "nogla" not in FLAGS:
                self.gla_chunk(kind, l, B, ci, c0, C, t0)
        if P and t0 + TT == SEQ:
            dst = self.glap_out.t[l].rearrange("h (dc p) e -> p (h dc) e", p=128)
            self.dma(V(self.glap_out, dst), self.S[l].v)
        if "noswa" in FLAGS:
            pass
        elif P:
            self.swa_prompt(l, B, t0, TT)
        else:
            self.swa_sample(l, B)
        for (off, fn, mode) in ((OFF_GR, AF.Silu, 0), (OFF_GA, AF.Sigmoid, 0), (OFF_GB, AF.Sigmoid, 1)):
            for g in range(4):
                s = self.slot()
                self.wload(s, self.w_in, l, off + g * 512, 512)

                def c_gate(cc, ps, g=g, fn=fn, mode=mode):
                    ch = g * 4 + cc
                    tmp = B["gtmp"][cc % 2]
                    self.act(tmp.v, ps, fn)
                    if mode == 0:
                        self.tt(B["onT"][:, ch, :], B["onT"][:, ch, :], tmp.v, ALU.mult)
                    else:
                        self.tt(tmp.v, tmp.v, B["oswaT"][:, ch, :], ALU.mult)
                        self.tt(B["onT"][:, ch, :], B["onT"][:, ch, :], tmp.v, ALU.add)
                self.proj_fm(s, 512, hT, TT, c_gate)
        if not P:
            self.dma(B["mG"].v, self.load_mod_s(B, l, 2))
        for g in range(4):
            s = self.slot()
            self.wload(s, self.w_out, l, g * 512, 512)
            self.proj_fm(s, 512, B["onT"], TT, lambda cc, ps, g=g: self.residual(kind, B, l, 1, g * 4 + cc, ps))

    def gla_chunk(self, kind, l, B, ci, c0, C, t0):
        P = kind == "p"
        nseq = 1 if P else NSEQ_S
        tri = self.cfv("trip") if P else self.cfv("tris", 64)
        dmat = self.cfv("dp") if P else self.cfv("ds", 64)
        mt = self.cfv("mtp") if P else self.cfv("mts", 64)
        ssel = self.cfv("sselp") if P else self.cfv("ssels", 64)
        first = P and t0 == 0 and ci == 0
        L_ = B["l"]
        eb, enb = B["eb"], B["enb"]
        for hf in range(2):
            ps = self.bank()
            self.mm(ps[0:C, :], B["glrT1"][:, c0:c0 + C], B["wal1"][:, hf * 512:(hf + 1) * 512])
            self.act(L_[:, hf * 512:(hf + 1) * 512], ps[0:C, :], AF.Exp, scale=-1.0)
        self.act(L_.v, L_.v, AF.Ln, bias=self.smallf[0:C, 1:2])
        psb = [self.bank(), self.bank()]
        for dc in range(8):
            self.mm(psb[dc // 4][:, (dc % 4) * C:(dc % 4 + 1) * C], L_[:, dc * 128:(dc + 1) * 128], tri)
        for hf in range(2):
            pv_ = psb[hf][:, 0:4 * C].re("p (a b) -> p a b", a=4)
            self.act(eb[:, hf * 4:(hf + 1) * 4, :], pv_, AF.Exp)
            self.act(enb[:, hf * 4:(hf + 1) * 4, :], pv_, AF.Exp, scale=-1.0)
        self.stt(eb.v, eb.v, 0.0625, B["qT"][:, :, c0:c0 + C], ALU.mult, ALU.mult)
        self.tt(enb.v, enb.v, B["kT"][:, :, c0:c0 + C], ALU.mult)
        pse = self.bank()
        for dc in range(8):
            self.mm(pse[:, dc * nseq:(dc + 1) * nseq], L_[:, dc * 128:(dc + 1) * 128], ssel)
        self.act(B["ebend"][:, :, 0:nseq], pse[:, 0:8 * nseq].re("p (a b) -> p a b", a=8), AF.Exp)
        pq = [self.bank(), self.bank()]
        for hf in range(2):
            self.mm(pq[hf][0:C, :], dmat, L_[:, hf * 512:(hf + 1) * 512])
        for hf in range(2):
            self.act(B["ebe"][:, hf * 512:(hf + 1) * 512], pq[hf][0:C, :], AF.Exp)
        self.tt(B["khat"].v, B["ktok"][:, ci, :], B["ebe"].v, ALU.mult)
        psa = self.bank()
        for h in range(4):
            for dc in range(2):
                self.mm(psa[0:C, h * C:(h + 1) * C], enb[:, 2 * h + dc, :], eb[:, 2 * h + dc, :],
                        start=(dc == 0), stop=(dc == 1))
        self.tt(B["attT"].v, psa[0:C, 0:4 * C].re("p (h c) -> p h c", h=4), mt.unsq(1).bc([C, 4, C]), ALU.mult)
        for h in range(4):
            if P:
                pso = self.bank()
                oviews = [pso[:, ec * C:(ec + 1) * C] for ec in range(4)]
                for ec in range(4):
                    self.mm(oviews[ec], B["vtok"][:, ci, h * 512 + ec * 128:h * 512 + (ec + 1) * 128],
                            B["attT"][:, h, :], start=True, stop=first)
                    if not first:
                        for dc in range(2):
                            self.mm(oviews[ec], self.S[l][:, 2 * h + dc, ec * 128:(ec + 1) * 128],
                                    eb[:, 2 * h + dc, :], start=False, stop=(dc == 1))
                excl = set()
            else:
                oviews = [self.banks[ec][:, 0:C] for ec in range(4)]
                excl = {0, 1, 2, 3}
                for ec in range(4):
                    self.mm(oviews[ec], B["vtok"][:, 0, h * 512 + ec * 128:h * 512 + (ec + 1) * 128],
                            B["attT"][:, h, :], start=True, stop=False)
                for b in range(NSEQ_S):
                    S0 = B["S0"][b % 2]
                    self.dma(S0.v, V(self.state_in, self.state_in.t[l, b, h].rearrange("(dc p) e -> p dc e", p=128)))
                    for ec in range(4):
                        for dc in range(2):
                            self.mm(self.banks[ec][:, LS * b:LS * b + LS], S0[:, dc, ec * 128:(ec + 1) * 128],
                                    eb[:, 2 * h + dc, LS * b:LS * b + LS], start=False,
                                    stop=(b == NSEQ_S - 1 and dc == 1))
                    khm = B["khm"][b % 2]
                    self.ts(khm[:, 0:256], B["khat"][:, h * 256:(h + 1) * 256],
                            self.cfv("ssel01", 64)[:, b:b + 1], ALU.mult)
                    for dc in range(2):
                        pss = self.bank_ex(excl)
                        self.mm(pss[:, :], khm[:, dc * 128:(dc + 1) * 128], B["vtok"][:, 0, h * 512:(h + 1) * 512])
                        sn = B["Snew"][dc]
                        self.stt(sn.v, S0[:, dc, :], B["ebend"][:, 2 * h + dc, b:b + 1], pss[:, :], ALU.mult, ALU.add)
                        self.dma(V(self.glas_out, self.glas_out.t[l, b, h, dc * 128:(dc + 1) * 128, :]), sn.v)
            osq = B["osq"]
            for ec in range(4):
                self.act(osq[:, ec, 0:C], oviews[ec], AF.Square)
            psn = self.bank_ex(excl)
            for ec in range(4):
                self.mm(psn[:, 0:C], self.cfv("ones"), osq[:, ec, 0:C], start=(ec == 0), stop=(ec == 3))
            ors = B["orstd"][:, 0:C]
            self.act(ors, psn[:, 0:C], AF.Ln, scale=1.0 / 512.0, bias=self.smallf[:, 0:1])
            self.act(ors, ors, AF.Exp, scale=-0.5)
            for ec in range(4):
                self.stt(B["onT"][:, 4 * h + ec, c0:c0 + C], oviews[ec],
                         self.pp[l][:, PP_GNG + ec:PP_GNG + ec + 1], ors, ALU.mult, ALU.mult)
            if P:
                for dc in range(2):
                    idx = 2 * h + dc
                    pss = self.bank()
                    self.mm(pss[:, :], B["khat"][:, idx * 128:(idx + 1) * 128], B["vtok"][:, ci, h * 512:(h + 1) * 512])
                    if first:
                        self.cp(self.S[l][:, idx, :], pss[:, :], eng="scalar")
                    else:
                        self.stt(self.S[l][:, idx, :], self.S[l][:, idx, :], B["ebend"][:, idx, 0:1], pss[:, :],
                                 ALU.mult, ALU.add)

    def swa_proj(self, l, B, TT, subt, c_kv):
        hT = B["hT"]
        if "sp_nosq" not in FLAGS:
            for g in range(4):
                s = self.slot()
                self.wload(s, self.w_in, l, OFF_SQ + g * 512, 512)
                self.proj_fm(s, 512, hT, TT, lambda cc, ps, g=g: self.cp(B["sqT"][:, g * 4 + cc, :], ps,
                                                                         eng="scalar" if cc % 2 else "vector"))
        s = self.slot()
        self.wload(s, self.w_in, l, OFF_SK, 512)
        if "sp_notm" not in FLAGS:
            self.proj_tm(s, 512, hT, subt, c_kv)
        if "sp_nodup" in FLAGS:
            return
        s2 = self.slot()
        for kv in range(4):
            for dup in range(2):
                self.cp(s2[:, :, kv * 128 + dup * 64:kv * 128 + dup * 64 + 64], s[:, :, kv * 64:(kv + 1) * 64],
                        eng="vector")
        self.proj_fm(s2, 512, hT, TT, lambda cc, ps: self.cp(B["skT"][:, cc, 128:128 + TT], ps, eng="scalar"))

    def swa_prompt(self, l, B, t0, TT):
        first_tile = t0 == 0
        last_tile = t0 + TT == SEQ
        subt = [(i * 128, 128) for i in range(4)]
        if not first_tile:
            self.cp(B["skT"][:, :, 0:128], self.pskT[l].v, eng="gpsimd")
            self.cp(B["sv1"][:, 0, :], self.psv[l].v, eng="gpsimd")

        def c_kv(si, ps):
            self.cp(B["sv1"][:, si + 1, :], ps[:, 256:512], eng="vector")
            if last_tile and si == 3:
                self.cp(B["kvo"].v, ps, eng="scalar")
                self.dma(self.kp_out[l], B["kvo"][:, 0:256])
                self.dma(self.vp_out[l], B["kvo"][:, 256:512])
        self.swa_proj(l, B, TT, subt, c_kv)
        if not last_tile:
            self.cp(self.pskT[l].v, B["skT"][:, :, TT:TT + 128], eng="gpsimd")
            self.cp(self.psv[l].v, B["sv1"][:, 4, :], eng="gpsimd")
        numb, denb = self.banks[0], self.banks[1]
        excl = {0, 1}
        onesb = self.cbv("ones")[:, 0:64]
        swam = self.cbv("swam")
        it = 0
        for chunk in range(16):
            kv = chunk // 4
            for half in range(2):
                pr = slice(64 * half, 64 * half + 64)
                for qb in range(4):
                    has_prev = not (first_tile and qb == 0)
                    ps = self.bank_ex(excl)
                    q = B["sqT"][pr, chunk, qb * 128:(qb + 1) * 128]
                    if has_prev:
                        self.mm(ps[:, 0:128], B["skT"][pr, kv, qb * 128:(qb + 1) * 128], q)
                    self.mm(ps[:, 128:256], B["skT"][pr, kv, (qb + 1) * 128:(qb + 2) * 128], q)
                    PT = B["PT"][it % 2]
                    it += 1
                    lo = 0 if has_prev else 128
                    self.act(PT[:, lo:256], ps[:, lo:256], AF.Exp, scale=0.125)
                    self.tt(PT[:, lo:256], PT[:, lo:256], swam[:, lo:256], ALU.mult, eng="gpsimd")
                    n_ = numb[pr, qb * 128:(qb + 1) * 128]
                    d_ = denb[pr, qb * 128:(qb + 1) * 128]
                    v_prev = B["sv1"][:, qb, kv * 64:(kv + 1) * 64]
                    v_cur = B["sv1"][:, qb + 1, kv * 64:(kv + 1) * 64]
                    if has_prev:
                        self.mm(n_, v_prev, PT[:, 0:128], start=True, stop=False)
                        self.mm(n_, v_cur, PT[:, 128:256], start=False, stop=True)
                        self.mm(d_, onesb, PT[:, 0:128], start=True, stop=False)
                        self.mm(d_, onesb, PT[:, 128:256], start=False, stop=True)
                    else:
                        self.mm(n_, v_cur, PT[:, 128:256])
                        self.mm(d_, onesb, PT[:, 128:256])
            g0 = B["gtmp"][chunk % 2]
            self.ts(g0.v, denb[:, 0:TT], self.esink[l][:, chunk:chunk + 1], ALU.add)
            self.recip(g0.v, g0.v)
            self.tt(B["oswaT"][:, chunk, :], numb[:, 0:TT], g0.v, ALU.mult)

    def swa_sample(self, l, B):
        TT = TS
        subt = [(0, TS)]

        def c_kv(si, ps):
            self.cp(B["sv1"][:, 1, :], ps[:, 256:512], eng="vector")
            self.cp(B["knew"].v, ps, eng="scalar")
        if "swa_noproj" not in FLAGS:
            self.swa_proj(l, B, TT, subt, c_kv)
        cksrc = self.ckT_in[l][:, :, 0:2048]
        if "swa_nocache" not in FLAGS:
            self.dma(B["ckT"][0:64].re("p (a c) b -> p a (c b)", c=16), cksrc, eng="gpsimd")
            self.dma(B["ckT"][64:128].re("p (a c) b -> p a (c b)", c=16), cksrc, eng="gpsimd")
        if "swa_nocv" not in FLAGS:
            self.dma(B["cv"].v, V(self.cv_in, self.cv_in.t[l].rearrange("b k c -> k b c")), eng="gpsimd")
        if "swa_nodd" not in FLAGS:
            self.dma(self.ks_out[l][:, 0:124, :], self.ck_in[l][:, 4:128, :])
            self.dma(self.vs_out[l][:, 0:124, :], self.cv_in[l][:, 4:128, :])
        for b in range(NSEQ_S if "swa_noout" not in FLAGS else 0):
            self.dma(self.ks_out[l][b, 124:128, :], B["knew"][LS * b:LS * b + LS, 0:256])
            self.dma(self.vs_out[l][b, 124:128, :], B["knew"][LS * b:LS * b + LS, 256:512])
        onesb = self.cbv("ones")[:, 0:64]
        it = 0
        for b in range(NSEQ_S if "swa_nobody" not in FLAGS else 0):
            for kv in range(4):
                pss = self.bank()
                psn_ = self.bank()
                for j in range(8):
                    hq = 8 * kv + j
                    chunk, half = hq // 2, hq % 2
                    pr = slice(64 * half, 64 * half + 64)
                    q = B["sqT"][pr, chunk, LS * b:LS * b + LS]
                    self.mm(pss[:, 4 * j:4 * j + 4], B["ckT"][pr, b * 4 + kv, :], q)
                    self.mm(psn_[0:TS, 4 * j:4 * j + 4], B["skT"][pr, kv, 128:128 + TS], q)
                PTs = B["PTs"][it % 2]
                PTn = B["PTn"][it % 2]
                it += 1
                self.act(PTs.v, pss[:, 0:32], AF.Exp, scale=0.125)
                self.tt(PTs.v, PTs.v, self.cbv("pastm"), ALU.mult, eng="gpsimd")
                self.act(PTn.v, psn_[0:TS, 0:32], AF.Exp, scale=0.125)
                self.tt(PTn.v, PTn.v, self.newm[:, b, :], ALU.mult, eng="gpsimd")
                nb = self.bank()
                db = self.bank()
                for half in range(2):
                    pr = slice(64 * half, 64 * half + 64)
                    pc = PTs.v.re("p (j i) -> p j i", i=4)[:, half::2, :]
                    nc_ = PTn.v.re("p (j i) -> p j i", i=4)[:, half::2, :]
                    n_ = nb[pr, 0:16].re("p (a b) -> p a b", b=4)
                    d_ = db[pr, 0:16].re("p (a b) -> p a b", b=4)
                    self.mm(n_, B["cv"][:, b, kv * 64:(kv + 1) * 64], pc, start=True, stop=False)
                    self.mm(n_, B["sv1"][:, 1, kv * 64:(kv + 1) * 64], nc_, start=False, stop=True)
                    self.mm(d_, onesb, pc, start=True, stop=False)
                    self.mm(d_, onesb[0:TS], nc_, start=False, stop=True)
                r3 = B["rden"][it % 2][:, 0:16].re("p (a b) -> p a b", b=4)
                self.tt(r3, db[:, 0:16].re("p (a b) -> p a b", b=4),
                        self.esink[l][:, 4 * kv:4 * kv + 4].unsq(2).bc([128, 4, 4]), ALU.add)
                self.recip(r3, r3)
                self.tt(B["oswaT"][:, 4 * kv:4 * kv + 4, LS * b:LS * b + LS],
                        nb[:, 0:16].re("p (a b) -> p a b", b=4), r3, ALU.mult)

    def peer(self, kind, t0, TT, l, B):
        P = kind == "p"
        subt = [(i * 128, 128) for i in range(4)] if P else [(0, TS)]
        self.norm_mod(kind, B, TT, l, 2)
        hT = B["hT"]
        if not P:
            self.dma(B["mG"].v, self.load_mod_s(B, l, 5))
        for g in range(4):
            s = self.slot()
            self.wload(s, self.peer_wq, l, g * 512, 512)
            self.proj_fm(s, 512, hT, TT, lambda cc, ps, g=g: self.cp(B["pqT"][:, g * 4 + cc, :], ps,
                                                                     eng="scalar" if cc % 2 else "vector"))
        self.dma(B["pkT"].v, self.pkT_in[l], eng="gpsimd")
        ident = self.cfv("ident")
        pm = B["pm"]
        pmb = B["pmb"]
        for si, (c0, n) in enumerate(subt):
            sc = B["sc"]
            for half in range(2):
                ps2 = [self.bank(), self.bank()]
                for q in range(8):
                    hc = half * 8 + q
                    self.mm(ps2[q // 4][0:n, (q % 4) * 128:(q % 4 + 1) * 128], B["pqT"][:, hc, c0:c0 + n],
                            B["pkT"][:, hc, :])
                for j in range(2):
                    self.cp(sc[:, half * 8 + j * 4:half * 8 + j * 4 + 4, :],
                            ps2[j][0:n, :].re("p (a b) -> p a b", a=4), eng="scalar")
            top16 = B["top16"]
            for hc in range(16):
                self.max8(top16[:, hc, 0:8], sc[:, hc, :])
                self.mrep(B["scr"].v, top16[:, hc, 0:8], sc[:, hc, :])
                self.max8(top16[:, hc, 8:16], B["scr"].v)
            c24 = B["c24"]
            cm = B["comb"]
            for h in range(8):
                self.tt(cm[0].v.re("p (a b) -> p a b", a=16), top16[:, 2 * h, :].unsq(2).bc([n, 16, 16]),
                        top16[:, 2 * h + 1, :].unsq(1).bc([n, 16, 16]), ALU.add)
                self.max8(c24[:, h, 0:8], cm[0].v)
                self.mrep(cm[1].v, c24[:, h, 0:8], cm[0].v)
                self.max8(c24[:, h, 8:16], cm[1].v)
                self.mrep(cm[2].v, c24[:, h, 8:16], cm[1].v)
                self.max8(c24[:, h, 16:24], cm[2].v)
            thr, negm, Z, gf = pm[:, 0:8], pm[:, 8:16], pm[:, 16:24], pm[:, 24:32]
            r0, r1, r2 = pm[:, 32:40], pm[:, 40:48], pm[:, 48:56]
            ex = pm[:, 64:192]
            th3 = pm[:, 192:216]
            th3v = th3.re("p (h k) -> p h k", k=3)
            self.tt(thr, c24[:, :, 15], c24[:, :, 16], ALU.add)
            self.ts(thr, thr, 0.5, ALU.mult)
            self.ts(negm, c24[:, :, 0], -1.0, ALU.mult)
            for h in range(8):
                self.act(ex[:, h * 16:(h + 1) * 16], c24[:, h, 0:16], AF.Exp, bias=negm[:, h:h + 1],
                         accum=Z[:, h:h + 1])
            self.tt(gf, thr, negm, ALU.add)
            self.act(gf, gf, AF.Exp)
            self.recip(Z, Z)
            self.tt(gf, gf, Z, ALU.mult)
            self.ts(r0, thr, -1.0, ALU.mult)
            self.cp(pmb[:, 0:8], r0)
            self.tt(r1, r0, pmb[:, 0:8], ALU.subtract)
            self.cp(pmb[:, 8:16], r1)
            self.tt(r2, r1, pmb[:, 8:16], ALU.subtract)
            self.cp(pmb[:, 16:24], r2)
            for kk in range(3):
                self.cp(th3v[:, :, kk], pmb[:, 8 * kk:8 * kk + 8])
            pst = self.bank()
            self.transpose(pst[0:24, 0:n], th3, ident[0:n, 0:n])
            self.cp(B["thT"][:, c0:c0 + n], pst[0:24, 0:n], eng="scalar")
            pst2 = self.bank()
            self.transpose(pst2[0:8, 0:n], gf, ident[0:n, 0:n])
            self.cp(B["gfT"][:, c0:c0 + n], pst2[0:8, 0:n], eng="scalar")
        for h in range(8):
            ps = self.bank()
            self.mm(ps[:, 0:TT], ident[0:8, h:h + 1].bc([8, 128]), B["gfT"][:, 0:TT])
            self.cp(B["gbc"][:, h, :], ps[:, 0:TT], eng="scalar")
        self.dump(f"gbc{kind}{l}_{t0}", B["gbc"].v)
        self.dump(f"thT{kind}{l}_{t0}", B["thT"].v, BF16)
        sel24 = self.cbv("sel24", 24)
        for blk in range(32):
            su = self.slot()
            self.wload(su, self.uT, l, blk * 512, 512)
            sv = self.slot()
            svv = sv.v.re("p a b -> p (a b)").re("p (a b) -> p a b", a=4)
            vsrc = V(self.pv, self.pv.t[l, blk * 512:(blk + 1) * 512, :].rearrange("(nc p) d -> p nc d", p=128))
            self.dma(svv, vsrc, eng="gpsimd")
            AT = B["AT"][blk % 2]
            hraw = B["hraw"]
            for nc_ in range(4):
                ps = self.bank()
                for kc in range(16):
                    self.mm(ps[:, 0:TT], su[:, kc, nc_ * 128:(nc_ + 1) * 128], hT[:, kc, 0:TT],
                            start=(kc == 0), stop=(kc == 15))
                self.cp(hraw[:, nc_, :], ps[:, 0:TT], eng="scalar")
            self.act(hraw.v, hraw.v, AF.Gelu)
            for nc_ in range(4):
                j = blk * 4 + nc_
                acc = B["acc"][nc_ % 2]
                for h in range(8):
                    pz = self.bank()
                    self.mm(pz[:, 0:TT], B["pkT"][:, 2 * h + 1, :], B["pqT"][:, 2 * h + 1, :], start=True, stop=False)
                    self.mm(pz[:, 0:TT], B["pkT"][:, 2 * h, j:j + 1].bc([128, 128]), B["pqT"][:, 2 * h, :],
                            start=False, stop=False)
                    self.mm(pz[:, 0:TT], sel24[:, h:h + 1].bc([24, 128]), B["thT"][:, 0:TT], start=False, stop=True)
                    zc = B["zc"][h % 2]
                    E = B["E"][h % 2]
                    self.act(zc.v, pz[:, 0:TT], AF.Prelu, alpha=1.0e7)
                    self.act(E.v, zc.v, AF.Exp)
                    if h == 0:
                        self.tt(acc.v, E.v, B["gbc"][:, 0, :], ALU.mult)
                    else:
                        tmp = B["ptmp"][h % 2]
                        self.tt(tmp.v, E.v, B["gbc"][:, h, :], ALU.mult)
                        self.tt(acc.v, acc.v, tmp.v, ALU.add)
                self.tt(AT[:, nc_, :], hraw[:, nc_, :], acc.v, ALU.mult)
            for dch in range(16):
                po = self.bank()
                for nc_ in range(4):
                    self.mm(po[:, 0:TT], svv[:, nc_, dch * 128:(dch + 1) * 128], AT[:, nc_, :],
                            start=(nc_ == 0), stop=(nc_ == 3))
                self.residual(kind, B, l, 2, dch, po[:, 0:TT])


_PROG = None


def _prep_inputs(inp):
    f = lambda a: np.ascontiguousarray(np.asarray(a, dtype=np.float32))
    cf, cb = make_consts()
    L = DEPTH
    shared = {
        "w_ada": f(inp["w_ada"]), "w_in": f(inp["w_in"]), "w_out": f(inp["w_out"]), "peer_wq": f(inp["peer_wq"]),
        "uT": f(np.transpose(np.asarray(inp["peer_u"]), (0, 2, 1))),
        "pv": f(inp["peer_v"]),
        "pkT": f(np.transpose(np.asarray(inp["peer_keys"]).reshape(L, 16, 128, 128), (0, 3, 1, 2))),
        "fg": f(np.asarray(inp["final_g"]).reshape(16, 128).T),
        "wal1": f(np.concatenate([np.asarray(inp["w_alpha2"]), np.asarray(inp["b_alpha"])[:, None, :]], axis=1)),
        "cf": cf, "cb": cb,
    }
    pp = np.zeros((L, 128, NPP), np.float32)
    for l in range(L):
        pp[l, :, PP_G1:PP_G1 + 16] = np.asarray(inp["norm1_g"])[l].reshape(16, 128).T
        pp[l, :, PP_G2:PP_G2 + 16] = np.asarray(inp["norm2_g"])[l].reshape(16, 128).T
        pp[l, :, PP_BADA:PP_BADA + 96] = np.asarray(inp["b_ada"])[l].reshape(96, 128).T
        pp[l, :, PP_GNG:PP_GNG + 4] = np.asarray(inp["gla_norm_g"])[l].reshape(4, 128).T
        sk = np.asarray(inp["swa_sinks"])[l]
        pp[l, :, PP_SINK:PP_SINK + 16] = np.repeat(sk.reshape(16, 2).T, 64, axis=0)
    shared["pp"] = pp
    xp = np.asarray(inp["x_prompt"])
    xs = np.asarray(inp["x_sample"])
    cp_ = np.asarray(inp["c_prompt"])
    cs = np.asarray(inp["c_sample"])
    st = np.asarray(inp["state_gla"])
    ck = np.asarray(inp["cache_swa_k"])
    cv = np.asarray(inp["cache_swa_v"])
    maps = []
    for i in range(NCORES):
        sl = slice(NSEQ_S * i, NSEQ_S * (i + 1))
        m = dict(shared)
        m["xT"] = f(np.concatenate([xp[i].T, xs[sl].reshape(TS, D).T], axis=1))
        m["cT"] = f(np.concatenate([cp_[i][None, :], np.repeat(cs[sl], LS, axis=0)], axis=0).T)
        m["state"] = f(st[:, sl])
        cki = ck[:, sl]
        ckt = np.zeros((L, 64, 4, 2048 + 64), np.float32)
        ckt[:, :, :, :2048] = np.transpose(cki, (0, 4, 1, 3, 2)).reshape(L, 64, 4, 2048)
        m["ckT"] = ckt
        m["ck"] = f(cki.reshape(L, NSEQ_S, 128, 256))
        m["cv"] = f(cv[:, sl].reshape(L, NSEQ_S, 128, 256))
        maps.append(m)
    return maps


def _assemble(res):
    L = DEPTH
    yp = np.stack([res[i]["yT"][:, :SEQ].T for i in range(NCORES)])
    ys = np.concatenate([res[i]["yT"][:, SEQ:].T.reshape(NSEQ_S, LS, D) for i in range(NCORES)])
    glap = np.stack([res[i]["gla_p"] for i in range(NCORES)], axis=1)
    kp = np.stack([res[i]["k_p"].reshape(L, 128, 4, 64) for i in range(NCORES)], axis=1)
    vp = np.stack([res[i]["v_p"].reshape(L, 128, 4, 64) for i in range(NCORES)], axis=1)
    glas = np.concatenate([res[i]["gla_s"] for i in range(NCORES)], axis=1)
    ks = np.concatenate([res[i]["k_s"].reshape(L, NSEQ_S, 128, 4, 64) for i in range(NCORES)], axis=1)
    vs = np.concatenate([res[i]["v_s"].reshape(L, NSEQ_S, 128, 4, 64) for i in range(NCORES)], axis=1)
    return tuple(np.ascontiguousarray(a, dtype=np.float32) for a in (yp, ys, glap, kp, vp, glas, ks, vs))


def kernel(**inputs):
    global _PROG
    if _PROG is None:
        _PROG = Prog()
    maps = _prep_inputs(inputs)
    r = run_bass_kernel_spmd(_PROG.nc, maps, core_ids=list(range(NCORES)))
    return _assemble(r.results)
```

```python
import numpy as np
import concourse.bass as bass
import concourse.mybir as mybir
from concourse.bass_utils import run_bass_kernel_spmd

F32 = mybir.dt.float32
BF16 = mybir.dt.bfloat16
U8 = mybir.dt.uint8
AF = mybir.ActivationFunctionType
ALU = mybir.AluOpType

NCORES = 8
D = 2048
DEPTH = 2
SEQ = 2048
NSEQ_S = 16
LS = 4
TS = NSEQ_S * LS
TTOK = SEQ + TS
INW = 12816
OFF_GQ, OFF_GK, OFF_GV, OFF_GR, OFF_GLR, OFF_SQ, OFF_SK, OFF_SV, OFF_GA, OFF_GB = (
    0, 1024, 2048, 4096, 6144, 6160, 8208, 8464, 8720, 10768)
EPS = 1e-6
EPOCH = 24000
NEG = -1.0e30
import os as _os
FLAGS = set(_os.environ.get("MKFLAGS", "").split(","))


class V:
    __slots__ = ("tile", "ap")

    def __init__(self, tile, ap):
        self.tile = tile
        self.ap = ap

    def __getitem__(self, idx):
        return V(self.tile, self.ap[idx])

    def bc(self, shape):
        return V(self.tile, self.ap.to_broadcast(list(shape)))

    def re(self, s, **kw):
        return V(self.tile, self.ap.rearrange(s, **kw))

    def unsq(self, ax):
        return V(self.tile, self.ap.unsqueeze(ax))

    @property
    def shape(self):
        return tuple(self.ap.shape)


class T:
    def __init__(self, t, name, space, rng=None):
        self.t = t
        self.name = name
        self.space = space
        self.last_w = None
        self.reads = []
        self.dsem = None
        self.dval = 0
        self.ro = False
        self.wo = False
        self.rng = rng
        self.aliases = []

    def __getitem__(self, idx):
        return V(self, self.t[idx])

    @property
    def v(self):
        return V(self, self.t)


class EngS:
    def __init__(self, name):
        self.name = name
        self.prog = []
        self.sems = []
        self.cnt = 0
        self.seen = {}


class K:
    def __init__(self, nc):
        self.nc = nc
        self.E = {n: EngS(n) for n in ("tensor", "vector", "scalar", "gpsimd", "sync")}
        self.nsem = 0
        self.ntile = 0
        self.dma_tiles = []
        self.swq = []
        self.arena = None
        self.carved = []

    def _sem(self, nm):
        self.nsem += 1
        return self.nc.alloc_semaphore(f"s{self.nsem}_{nm}"[:40])

    def make_arena(self, nbytes):
        self.arena = self.nc.alloc_sbuf_tensor("arena", [128, nbytes], U8)
        self.arena_bytes = nbytes

    def carve(self, off, shape, dt, name):
        isz = 4 if dt == F32 else (2 if dt == BF16 else 1)
        n = 1
        for s in shape[1:]:
            n *= s
        nb = n * isz
        assert off % 4 == 0 and off + nb <= self.arena_bytes, (name, off, nb, self.arena_bytes)
        ap = self.arena[0:shape[0], off:off + nb].bitcast(dt)
        if len(shape) == 3:
            ap = ap.rearrange("p (a b) -> p a b", a=shape[1])
        elif len(shape) == 4:
            ap = ap.rearrange("p (a b c) -> p a b c", a=shape[1], b=shape[2])
        self.ntile += 1
        t = T(ap, f"{name}_{self.ntile}", "sb", (off, off + nb))
        for o in self.carved:
            if o.rng[0] < t.rng[1] and t.rng[0] < o.rng[1]:
                o.aliases.append(t)
                t.aliases.append(o)
        self.carved.append(t)
        return t

    def ps(self, name):
        self.ntile += 1
        h = self.nc.alloc_psum_tensor(f"{name}_{self.ntile}", [128, 512], F32)
        return T(h[:, :], name, "ps")

    def dram(self, name, shape, dt, kind):
        h = self.nc.dram_tensor(name, list(shape), dt, kind=kind)
        t = T(h.ap(), name, "dram")
        if kind == "ExternalInput":
            t.ro = True
        if kind == "ExternalOutput":
            t.wo = True
        return t

    def _need(self, E, sem, val):
        key = id(sem)
        if val <= 0 or E.seen.get(key, 0) >= val:
            return
        E.seen[key] = val
        E.prog.append(("wait", sem, val))

    def _deps(self, E, reads, writes):
        for t0 in reads:
            if t0.ro:
                continue
            for t in [t0] + t0.aliases:
                if t.last_w is not None:
                    sem, val, en = t.last_w
                    if not (en == E.name and en == "tensor"):
                        self._need(E, sem, val)
                if t.space == "ps":
                    for (sem, val, en) in t.reads:
                        if en != E.name:
                            self._need(E, sem, val)
        for t0 in writes:
            for t in [t0] + t0.aliases:
                if t.last_w is not None:
                    sem, val, en = t.last_w
                    if en != E.name:
                        self._need(E, sem, val)
                for (sem, val, en) in t.reads:
                    if en != E.name:
                        self._need(E, sem, val)

    def op(self, eng, fn, reads=(), writes=()):
        E = self.E[eng]
        self._deps(E, reads, writes)
        if not E.sems or E.cnt >= EPOCH:
            E.sems.append(self._sem(eng))
            E.cnt = 0
        E.cnt += 1
        sem = E.sems[-1]
        E.prog.append(("op", fn, sem))
        rec = (sem, E.cnt, eng)
        for t in reads:
            if t.ro:
                continue
            t.reads.append(rec)
            if len(t.reads) > 48:
                d = {}
                for r in t.reads:
                    kk = id(r[0])
                    if kk not in d or d[kk][1] < r[1]:
                        d[kk] = r
                t.reads = list(d.values())
        for t in writes:
            t.last_w = rec
            t.reads = []

    def dma(self, eng, out, in_):
        E = self.E[eng]
        out_t, in_t = out.tile, in_.tile
        if eng == "gpsimd":
            def ndesc(ap):
                n = 1
                for d in ap.shape[:-1]:
                    n *= d
                return n
            nd = (max(ndesc(out.ap), ndesc(in_.ap)) + 15) // 16 + 2
            fifo = self.swq
            while fifo and sum(x[2] for x in fifo) + nd > 640:
                sem0, val0, _ = fifo.pop(0)
                self._need(E, sem0, val0)
        self._deps(E, [in_t], [out_t])
        sb_t = out_t if out_t.space != "dram" else in_t
        if sb_t.dsem is None:
            sb_t.dsem = self._sem("d" + sb_t.name)
            self.dma_tiles.append(sb_t)
        sb_t.dval += 16
        sem, val = sb_t.dsem, sb_t.dval
        E.prog.append(("dma", out.ap, in_.ap, sem))
        if eng == "gpsimd":
            self.swq.append((sem, val, nd))
        if not out_t.ro and not out_t.wo:
            out_t.last_w = (sem, val, "dma")
            out_t.reads = []
        if not in_t.ro:
            in_t.reads.append((sem, val, "dma"))

    def emit(self):
        nc = self.nc
        S = self.E["sync"]
        for t in self.dma_tiles:
            self._need(S, t.dsem, t.dval)
        with nc.Block() as block:
            def run(E, eng):
                for ent in E.prog:
                    if ent[0] == "wait":
                        eng.wait_ge(ent[1], ent[2])
                    elif ent[0] == "op":
                        ent[1](eng).then_inc(ent[2], 1)
                    else:
                        eng.dma_start(out=ent[1], in_=ent[2]).then_inc(ent[3], 16)

            @block.tensor
            def _(e):
                run(self.E["tensor"], e)

            @block.vector
            def _(e):
                run(self.E["vector"], e)

            @block.scalar
            def _(e):
                run(self.E["scalar"], e)

            @block.gpsimd
            def _(e):
                run(self.E["gpsimd"], e)

            @block.sync
            def _(e):
                run(self.E["sync"], e)

    def stats(self):
        return ({n: sum(1 for x in e.prog if x[0] != "wait") for n, e in self.E.items()},
                {n: sum(1 for x in e.prog if x[0] == "wait") for n, e in self.E.items()}, self.nsem)


CF = {}
_o = 0
for _n, _w in [("ident", 128), ("ones", 128), ("trip", 128), ("dp", 128), ("mtp", 128),
               ("tris", 64), ("ds", 64), ("mts", 64), ("ssels", 16), ("ssel01", 16), ("sselp", 1),
               ("blk01s", 64)]:
    CF[_n] = (_o, _w)
    _o += _w
NCF = _o
CB = {}
_o = 0
for _n, _w in [("ones", 128), ("swam", 256), ("sel24", 8), ("pastm", 32), ("newm", 512)]:
    CB[_n] = (_o, _w)
    _o += _w
NCB = _o


def make_consts():
    cf = np.zeros((128, NCF), np.float32)
    cb = np.zeros((128, NCB), np.float32)

    def put(dst, tab, name, arr):
        o, w = tab[name]
        dst[:arr.shape[0], o:o + w] = arr

    i = np.arange(128)
    put(cf, CF, "ident", np.eye(128, dtype=np.float32))
    put(cf, CF, "ones", np.ones((128, 128), np.float32))
    le = (i[:, None] <= i[None, :]).astype(np.float32)
    gt = (i[:, None] > i[None, :]).astype(np.float32)
    put(cf, CF, "trip", le * (-1.0 / 16.0))
    put(cf, CF, "dp", gt * (-1.0 / 16.0))
    put(cf, CF, "mtp", le)
    put(cf, CF, "sselp", np.full((128, 1), -1.0 / 16.0, np.float32))
    j = np.arange(64)
    same = (j[:, None] // 4 == j[None, :] // 4)
    les = (same & (j[:, None] <= j[None, :])).astype(np.float32)
    gts = (same & (j[:, None] > j[None, :])).astype(np.float32)
    put(cf, CF, "tris", les * (-1.0 / 16.0))
    put(cf, CF, "ds", gts * (-1.0 / 16.0))
    put(cf, CF, "mts", les)
    sel = (j[:, None] // 4 == np.arange(16)[None, :]).astype(np.float32)
    put(cf, CF, "ssels", sel * (-1.0 / 16.0))
    put(cf, CF, "ssel01", sel)
    put(cf, CF, "blk01s", same.astype(np.float32))
    put(cb, CB, "ones", np.ones((128, 128), np.float32))
    put(cb, CB, "swam", np.concatenate([gt, le], axis=1))
    s24 = np.zeros((24, 8), np.float32)
    for h in range(8):
        s24[3 * h:3 * h + 3, h] = 1.0
    put(cb, CB, "sel24", s24)
    pm = (i[:, None] > (np.arange(32)[None, :] % 4)).astype(np.float32)
    put(cb, CB, "pastm", pm)
    nm = np.zeros((64, 16, 32), np.float32)
    for s_ in range(64):
        for c in range(32):
            if (s_ % 4) <= (c % 4):
                nm[s_, s_ // 4, c] = 1.0
    put(cb, CB, "newm", nm.reshape(64, 512))
    return cf, cb


NPP = 16 + 16 + 96 + 4 + 16
PP_G1, PP_G2, PP_BADA, PP_GNG, PP_SINK = 0, 16, 32, 128, 132


class Prog:
    def __init__(self, dbg=()):
        self.dbg = set(dbg)
        self.dbg_out = {}
        nc = bass.Bass("TRN2", target_bir_lowering=False)
        self.nc = nc
        k = K(nc)
        self.k = k
        dr = k.dram
        self.xT_in = dr("xT", [D, TTOK], F32, "ExternalInput")
        self.cT_in = dr("cT", [D, 1 + TS], F32, "ExternalInput")
        self.state_in = dr("state", [DEPTH, NSEQ_S, 4, 256, 512], F32, "ExternalInput")
        self.ckT_in = dr("ckT", [DEPTH, 64, 4, 2048 + 64], F32, "ExternalInput")
        self.ck_in = dr("ck", [DEPTH, NSEQ_S, 128, 256], F32, "ExternalInput")
        self.cv_in = dr("cv", [DEPTH, NSEQ_S, 128, 256], F32, "ExternalInput")
        self.w_ada = dr("w_ada", [DEPTH, D, 6 * D], F32, "ExternalInput")
        self.w_in = dr("w_in", [DEPTH, D, INW], F32, "ExternalInput")
        self.w_out = dr("w_out", [DEPTH, D, D], F32, "ExternalInput")
        self.peer_wq = dr("peer_wq", [DEPTH, D, D], F32, "ExternalInput")
        self.uT = dr("uT", [DEPTH, D, 16384], F32, "ExternalInput")
        self.pv = dr("pv", [DEPTH, 16384, D], F32, "ExternalInput")
        self.pkT_in = dr("pkT", [DEPTH, 128, 16, 128], F32, "ExternalInput")
        self.pp_in = dr("pp", [DEPTH, 128, NPP], F32, "ExternalInput")
        self.fg_in = dr("fg", [128, 16], F32, "ExternalInput")
        self.wal_in = dr("wal1", [DEPTH, 17, 1024], F32, "ExternalInput")
        self.cf_in = dr("cf", [128, NCF], F32, "ExternalInput")
        self.cb_in = dr("cb", [128, NCB], F32, "ExternalInput")
        self.yT_out = dr("yT", [D, TTOK], F32, "ExternalOutput")
        self.glap_out = dr("gla_p", [DEPTH, 4, 256, 512], F32, "ExternalOutput")
        self.kp_out = dr("k_p", [DEPTH, 128, 256], F32, "ExternalOutput")
        self.vp_out = dr("v_p", [DEPTH, 128, 256], F32, "ExternalOutput")
        self.glas_out = dr("gla_s", [DEPTH, NSEQ_S, 4, 256, 512], F32, "ExternalOutput")
        self.ks_out = dr("k_s", [DEPTH, NSEQ_S, 128, 256], F32, "ExternalOutput")
        self.vs_out = dr("v_s", [DEPTH, NSEQ_S, 128, 256], F32, "ExternalOutput")
        self.mods = dr("mods", [DEPTH, 128, 96, TS], F32, "Internal")

        self.banks = [k.ps(f"bank{i}") for i in range(8)]
        self.bank_i = 0
        self.slot_i = 0
        self.alloc()
        self.build()
        k.emit()

    def bank(self):
        b = self.banks[self.bank_i % 8]
        self.bank_i += 1
        return b

    def mm(self, out, lhsT, rhs, start=True, stop=True):
        self.k.op("tensor", lambda e: e.matmul(out.ap, lhsT=lhsT.ap, rhs=rhs.ap, start=start, stop=stop),
                  [lhsT.tile, rhs.tile], [out.tile])

    def transpose(self, out, in_, ident):
        self.k.op("tensor", lambda e: e.transpose(out.ap, in_.ap, ident.ap), [in_.tile, ident.tile], [out.tile])

    def act(self, out, in_, func, scale=None, bias=None, alpha=None, accum=None):
        kw = {}
        reads = [in_.tile]
        writes = [out.tile]
        if scale is not None:
            if isinstance(scale, V):
                kw["scale"] = scale.ap
                reads.append(scale.tile)
            else:
                kw["scale"] = float(scale)
        if bias is not None:
            if isinstance(bias, V):
                kw["bias"] = bias.ap
                reads.append(bias.tile)
            else:
                kw["bias"] = float(bias)
        if alpha is not None:
            kw["alpha"] = float(alpha)
        if accum is not None:
            kw["accum_out"] = accum.ap
            writes.append(accum.tile)
        self.k.op("scalar", lambda e: e.activation(out=out.ap, in_=in_.ap, func=func, **kw), reads, writes)

    def tt(self, out, a, b, op, eng="vector"):
        self.k.op(eng, lambda e: e.tensor_tensor(out=out.ap, in0=a.ap, in1=b.ap, op=op),
                  [a.tile, b.tile], [out.tile])

    def ts(self, out, a, s1, op0, s2=None, op1=None, eng="vector"):
        reads = [a.tile]
        a1 = s1
        a2 = s2
        if isinstance(s1, V):
            reads.append(s1.tile)
            a1 = s1.ap
        if isinstance(s2, V):
            reads.append(s2.tile)
            a2 = s2.ap
        if op1 is None:
            self.k.op(eng, lambda e: e.tensor_scalar(out=out.ap, in0=a.ap, scalar1=a1, scalar2=None, op0=op0),
                      reads, [out.tile])
        else:
            self.k.op(eng, lambda e: e.tensor_scalar(out=out.ap, in0=a.ap, scalar1=a1, scalar2=a2, op0=op0, op1=op1),
                      reads, [out.tile])

    def stt(self, out, a, s, b, op0, op1):
        reads = [a.tile, b.tile]
        sa = s
        if isinstance(s, V):
            reads.append(s.tile)
            sa = s.ap
        self.k.op("vector", lambda e: e.scalar_tensor_tensor(out=out.ap, in0=a.ap, scalar=sa, in1=b.ap, op0=op0, op1=op1),
                  reads, [out.tile])

    def cp(self, out, in_, eng="vector"):
        if eng == "scalar":
            self.act(out, in_, AF.Copy)
        else:
            self.k.op(eng, lambda e: e.tensor_copy(out=out.ap, in_=in_.ap), [in_.tile], [out.tile])

    def memset(self, out, val, eng="vector"):
        self.k.op(eng, lambda e: e.memset(out.ap, val), [], [out.tile])

    def recip(self, out, in_):
        self.k.op("vector", lambda e: e.reciprocal(out=out.ap, in_=in_.ap), [in_.tile], [out.tile])

    def max8(self, out, in_):
        self.k.op("vector", lambda e: e.max(out=out.ap, in_=in_.ap), [in_.tile], [out.tile])

    def mrep(self, out, rep, vals):
        self.k.op("vector", lambda e: e.match_replace(out=out.ap, in_to_replace=rep.ap, in_values=vals.ap, imm_value=NEG),
                  [rep.tile, vals.tile], [out.tile])

    def dma(self, out, in_, eng="sync"):
        self.k.dma(eng, out, in_)

    def dump(self, name, v, dt=F32):
        if name not in self.dbg:
            return
        shp = list(v.shape)
        o = self.k.dram("dbg_" + name, shp, dt, "ExternalOutput")
        self.dbg_out[name] = o
        self.dma(o.v, v, eng="gpsimd" if dt != v.ap.dtype else "sync")

    def alloc(self):
        k = self.k
        AB = 206 * 1024
        k.make_arena(AB)
        o = 0

        def take(nb):
            nonlocal o
            r = o
            o += (nb + 31) // 32 * 32
            return r

        c = k.carve
        self.cf = c(take(NCF * 4), [128, NCF], F32, "cf")
        self.cb = c(take(NCB * 2), [128, NCB], BF16, "cb")
        self.S = [c(take(16384), [128, 8, 512], F32, f"S{l}") for l in range(DEPTH)]
        self.pskT = [c(take(1024), [128, 4, 128], BF16, f"pskT{l}") for l in range(DEPTH)]
        self.psv = [c(take(512), [128, 256], BF16, f"psv{l}") for l in range(DEPTH)]
        self.pp = [c(take(NPP * 4), [128, NPP], F32, f"pp{l}") for l in range(DEPTH)]
        self.fg = c(take(64), [128, 16], F32, "fg")
        self.modp = [c(take(96 * 4), [128, 96], F32, f"modp{l}") for l in range(DEPTH)]
        self.A1 = [c(take(64), [128, 16], F32, f"A1{l}") for l in range(DEPTH)]
        self.A2 = [c(take(64), [128, 16], F32, f"A2{l}") for l in range(DEPTH)]
        self.esink = [c(take(64), [128, 16], F32, f"esink{l}") for l in range(DEPTH)]
        self.smallf = c(take(1024), [128, 256], F32, "smallf")
        self.slots = [c(take(16384), [128, 16, 512], BF16, f"slot{i}") for i in range(2)]
        self.base = o
        self.layouts = {512: self.layout(512), 64: self.layout(64)}
        self.newm = self.cbv("newm", 64).re("p (b c) -> p b c", b=16)

    def layout(self, TT):
        k = self.k
        c = k.carve
        o = self.base
        NST = 1 if TT == 64 else 4
        TP = min(TT, 128)
        B = {}

        def take(nb):
            nonlocal o
            r = o
            o += (nb + 31) // 32 * 32
            return r

        B["xT"] = c(take(16 * TT * 4), [128, 16, TT], F32, "xT")
        B["hT"] = c(take(16 * TT * 2), [128, 16, TT], BF16, "hT")
        ph = o
        B["sq"] = [c(take(TT * 4), [128, TT], F32, "sq") for _ in range(2)]
        B["rstd"] = c(take(TT * 4), [128, TT], F32, "rstd")
        B["ntmp"] = [c(take(TT * 4), [128, TT], F32, "ntmp") for _ in range(2)]
        o = ph
        B["qT"] = c(take(8 * TT * 2), [128, 8, TT], BF16, "qT")
        B["kT"] = c(take(8 * TT * 2), [128, 8, TT], BF16, "kT")
        B["ktok"] = c(take(NST * 1024 * 2), [TP, NST, 1024], BF16, "ktok")
        vt_off = take(NST * 2048 * 2)
        B["vtok"] = c(vt_off, [TP, NST, 2048], BF16, "vtok")
        B["onT"] = c(take(16 * TT * 2), [128, 16, TT], BF16, "onT")
        B["glrT1"] = c(take(TT * 2), [17, TT], BF16, "glrT1")
        B["wal1"] = c(take(1024 * 2), [17, 1024], BF16, "wal1")
        tr = o
        B["l"] = c(take(4096), [TP, 1024], F32, "l")
        B["ebe"] = B["l"]
        B["eb"] = c(take(8 * TP * 4), [128, 8, TP], F32, "eb")
        B["enb"] = c(take(8 * TP * 4), [128, 8, TP], F32, "enb")
        B["khat"] = c(take(2048), [TP, 1024], BF16, "khat")
        B["attT"] = c(take(4 * TP * 2), [TP, 4, TP], BF16, "attT")
        B["osq"] = c(take(4 * TP * 4), [128, 4, TP], F32, "osq")
        B["orstd"] = c(take(TP * 4), [128, TP], F32, "orstd")
        B["ebend"] = c(take(8 * 16 * 4), [128, 8, 16], F32, "ebend")
        if TT == 64:
            B["S0"] = [c(take(4096), [128, 2, 512], F32, "S0") for _ in range(2)]
            B["Snew"] = [c(take(2048), [128, 512], F32, "Snew") for _ in range(2)]
            B["khm"] = [c(take(2048), [TP, 1024], BF16, "khm") for _ in range(2)]
            B["ckT"] = c(take(64 * 128 * 2), [128, 64, 128], BF16, "ckT")
            B["cv"] = c(take(16 * 256 * 2), [128, 16, 256], BF16, "cv")
            B["knew"] = c(take(512 * 4), [TP, 512], F32, "knew")
            B["mA"] = c(take(16 * TT * 4), [128, 16, TT], F32, "mA")
            B["mS"] = c(take(16 * TT * 4), [128, 16, TT], F32, "mS")
            B["mG"] = c(take(16 * TT * 4), [128, 16, TT], F32, "mG")
            B["PTs"] = [c(take(64), [128, 32], BF16, "PTs") for _ in range(2)]
            B["PTn"] = [c(take(64), [TP, 32], BF16, "PTn") for _ in range(2)]
            B["rtmp"] = [c(take(TT * 4), [128, TT], F32, "rtmp") for _ in range(2)]
            tr = o
        end_mixer = o
        o = ph
        B["sqT"] = c(take(16 * TT * 2), [128, 16, TT], BF16, "sqT")
        o = vt_off
        B["oswaT"] = c(take(16 * TT * 2), [128, 16, TT], BF16, "oswaT")
        o = tr
        B["skT"] = c(take(4 * (128 + TT) * 2), [128, 4, 128 + TT], BF16, "skT")
        B["sv1"] = c(take((NST + 1) * 256 * 2), [TP, NST + 1, 256], BF16, "sv1")
        B["PT"] = [c(take(512), [128, 256], BF16, "PT") for _ in range(2)]
        B["kvo"] = c(take(512 * 4), [TP, 512], F32, "kvo")
        B["rden"] = [c(take(512), [128, 128], F32, "rden") for _ in range(2)]
        B["gtmp"] = [c(take(TT * 4), [128, TT], F32, "gtmp") for _ in range(2)]
        end_mixer = max(end_mixer, o)
        o = ph
        B["pqT"] = c(take(16 * TT * 2), [128, 16, TT], BF16, "pqT")
        B["gbc"] = c(take(8 * TT * 4), [128, 8, TT], F32, "gbc")
        B["gbcb"] = c(take(8 * TT * 2), [128, 8, TT], BF16, "gbcb")
        B["pkT"] = c(take(16 * 128 * 2), [128, 16, 128], BF16, "pkT")
        B["thT"] = c(take(TT * 2), [128, TT], BF16, "thT")
        B["gfT"] = c(take(TT * 4), [8, TT], F32, "gfT")
        pk = o
        B["sc"] = c(take(16 * 128 * 4), [TP, 16, 128], F32, "sc")
        B["scr"] = c(take(512), [TP, 128], F32, "scr")
        B["top16"] = c(take(16 * 16 * 4), [TP, 16, 16], F32, "top16")
        B["comb"] = [c(take(1024), [TP, 256], F32, "comb") for _ in range(3)]
        B["c24"] = c(take(8 * 24 * 4), [TP, 8, 24], F32, "c24")
        B["pm"] = c(take(256 * 4), [TP, 256], F32, "pm")
        B["pmb"] = c(take(64 * 2), [TP, 64], BF16, "pmb")
        o = pk
        B["hraw"] = c(take(4 * TT * 4), [128, 4, TT], F32, "hraw")
        B["zc"] = [c(take(TT * 4), [128, TT], F32, "zc") for _ in range(2)]
        B["E"] = [c(take(TT * 2), [128, TT], BF16, "E") for _ in range(4)]
        B["ptmp"] = [c(take(TT * 2), [128, TT], BF16, "ptmp") for _ in range(4)]
        B["acc"] = [c(take(TT * 2), [128, TT], BF16, "acc") for _ in range(2)]
        B["AT"] = [c(take(4 * TT * 2), [128, 4, TT], BF16, "AT") for _ in range(2)]
        end_peer = o
        B["_end"] = max(end_mixer, end_peer)
        return B

    def slot(self):
        s = self.slots[self.slot_i % 2]
        self.slot_i += 1
        return s

    def wload(self, s, W, l, c0, n, dst=0):
        src = V(W, W.t[l].rearrange("(kc p) n -> p kc n", p=128)[:, :, c0:c0 + n])
        self.dma(s[:, :, dst:dst + n], src, eng="gpsimd")

    def proj_fm(self, s, ncols, hT, TT, consume, col0=0):
        nch = (ncols + 127) // 128
        for cc in range(nch):
            M = min(128, ncols - cc * 128)
            ps = self.bank()
            for kc in range(16):
                self.mm(ps[0:M, 0:TT], s[:, kc, col0 + cc * 128:col0 + cc * 128 + M], hT[:, kc, 0:TT],
                        start=(kc == 0), stop=(kc == 15))
            consume(cc, ps[0:M, 0:TT])

    def proj_tm(self, s, ncols, hT, subt, consume):
        for si, (t0, n) in enumerate(subt):
            ps = self.bank()
            for kc in range(16):
                self.mm(ps[0:n, 0:ncols], hT[:, kc, t0:t0 + n], s[:, kc, 0:ncols], start=(kc == 0), stop=(kc == 15))
            consume(si, ps[0:n, 0:ncols])

    def cfv(self, name, rows=128):
        o, w = CF[name]
        return self.cf[0:rows, o:o + w]

    def cbv(self, name, rows=128):
        o, w = CB[name]
        return self.cb[0:rows, o:o + w]

    def build(self):
        self.dma(self.cf.v, self.cf_in.v)
        self.dma(self.cb.v, self.cb_in.v, eng="gpsimd")
        self.dma(self.fg.v, self.fg_in.v)
        for l in range(DEPTH):
            self.dma(self.pp[l].v, self.pp_in[l])
            self.act(self.esink[l].v, self.pp[l][:, PP_SINK:PP_SINK + 16], AF.Exp)
        self.memset(self.smallf[:, 0:1], EPS)
        self.memset(self.smallf[:, 1:2], 1.0)
        self.compute_mod()
        for l in range(DEPTH):
            self.dump(f"modp{l}", self.modp[l].v)
        tiles = [("s", SEQ, TS)] + [("p", i * 512, 512) for i in range(4)]
        if "tiles_s" in FLAGS:
            tiles = tiles[:1]
        if "tiles_p0" in FLAGS:
            tiles = tiles[1:2]
        if "tiles_sp0" in FLAGS:
            tiles = tiles[0:2]
        for (kind, t0, TT) in tiles:
            self.run_tile(kind, t0, TT)

    def compute_mod(self):
        B = self.layouts[512]
        NCOL = 1 + TS
        cs = B["xT"]
        sc = B["hT"]
        stage = B["onT"]
        stg = B["gbc"]
        self.dma(cs[:, :, 0:NCOL], V(self.cT_in, self.cT_in.t.rearrange("(kc p) n -> p kc n", p=128)))
        self.act(sc[:, :, 0:NCOL], cs[:, :, 0:NCOL], AF.Silu)
        for l in range(DEPTH):
            for g in range(24):
                s = self.slot()
                self.wload(s, self.w_ada, l, g * 512, 512)

                def consume(cc, ps, g=g, l=l):
                    ch = g * 4 + cc
                    self.act(self.modp[l][:, ch:ch + 1], ps[:, 0:1], AF.Identity,
                             bias=self.pp[l][:, PP_BADA + ch:PP_BADA + ch + 1])
                    self.act(stg[:, ch % 8, 0:TS], ps[:, 1:NCOL], AF.Identity,
                             bias=self.pp[l][:, PP_BADA + ch:PP_BADA + ch + 1])
                    if ch % 8 == 7:
                        c8 = ch - 7
                        self.dma(self.mods[l][:, c8:c8 + 8, :], stg[:, :, 0:TS])
                self.proj_fm(s, 512, sc, NCOL, consume)
            self.stt(self.A1[l].v, self.modp[l][:, 16:32], 1.0, self.pp[l][:, PP_G1:PP_G1 + 16], ALU.add, ALU.mult)
            self.stt(self.A2[l].v, self.modp[l][:, 64:80], 1.0, self.pp[l][:, PP_G2:PP_G2 + 16], ALU.add, ALU.mult)

    def run_tile(self, kind, t0, TT):
        B = self.layouts[TT]
        xsrc = V(self.xT_in, self.xT_in.t.rearrange("(c p) t -> p c t", p=128)[:, :, t0:t0 + TT])
        self.dma(B["xT"].v, xsrc)
        for l in range(1 if "depth1" in FLAGS else DEPTH):
            self.layer(kind, t0, TT, l, B)
        self.final_norm(kind, t0, TT, B)

    def rms_rstd(self, B, TT):
        xT = B["xT"]
        ps = self.bank()
        for c in range(16):
            sq = B["sq"][c % 2]
            self.act(sq.v, xT[:, c, :], AF.Square)
            self.mm(ps[:, 0:TT], self.cfv("ones"), sq.v, start=(c == 0), stop=(c == 15))
        self.act(B["rstd"].v, ps[:, 0:TT], AF.Ln, scale=1.0 / D, bias=self.smallf[:, 0:1])
        self.act(B["rstd"].v, B["rstd"].v, AF.Exp, scale=-0.5)

    def load_mod_s(self, B, l, which):
        return V(self.mods, self.mods.t[l][:, which * 16:(which + 1) * 16, :])

    def norm_mod(self, kind, B, TT, l, which):
        self.rms_rstd(B, TT)
        xT, hT = B["xT"], B["hT"]
        if kind == "p":
            A = (self.A1 if which == 1 else self.A2)[l]
            sh0 = 0 if which == 1 else 48
            for c in range(16):
                tmp = B["ntmp"][c % 2]
                self.stt(tmp.v, xT[:, c, :], A[:, c:c + 1], B["rstd"].v, ALU.mult, ALU.mult)
                self.act(hT[:, c, :], tmp.v, AF.Identity, bias=self.modp[l][:, sh0 + c:sh0 + c + 1])
        else:
            g0 = PP_G1 if which == 1 else PP_G2
            sc_i, sh_i = (1, 0) if which == 1 else (4, 3)
            self.dma(B["mA"].v, self.load_mod_s(B, l, sc_i))
            self.dma(B["mS"].v, self.load_mod_s(B, l, sh_i))
            for c in range(16):
                self.ts(B["mA"][:, c, :], B["mA"][:, c, :], 1.0, ALU.add,
                        self.pp[l][:, g0 + c:g0 + c + 1], ALU.mult)
                tmp = B["ntmp"][c % 2]
                self.tt(tmp.v, xT[:, c, :], B["rstd"].v, ALU.mult)
                self.tt(tmp.v, tmp.v, B["mA"][:, c, :], ALU.mult)
                self.tt(hT[:, c, :], tmp.v, B["mS"][:, c, :], ALU.add)

    def residual(self, kind, B, l, which, c, ps):
        xT = B["xT"]
        if kind == "p":
            g0 = 32 if which == 1 else 80
            self.stt(xT[:, c, :], ps, self.modp[l][:, g0 + c:g0 + c + 1], xT[:, c, :], ALU.mult, ALU.add)
        else:
            tmp = B["rtmp"][c % 2]
            self.tt(tmp.v, ps, B["mG"][:, c, :], ALU.mult)
            self.tt(xT[:, c, :], xT[:, c, :], tmp.v, ALU.add)

    def final_norm(self, kind, t0, TT, B):
        self.rms_rstd(B, TT)
        ydst = self.yT_out.t.rearrange("(c p) t -> p c t", p=128)
        for c in range(16):
            tmp = B["ntmp"][c % 2]
            self.stt(tmp.v, B["xT"][:, c, :], self.fg[:, c:c + 1], B["rstd"].v, ALU.mult, ALU.mult)
            self.dma(V(self.yT_out, ydst[:, c, t0:t0 + TT]), tmp.v)

    def bank_ex(self, exclude):
        while True:
            b = self.banks[self.bank_i % 8]
            self.bank_i += 1
            if (self.bank_i - 1) % 8 not in exclude:
                return b

    def layer(self, kind, t0, TT, l, B):
        if "nomixer" not in FLAGS:
            self.mixer(kind, t0, TT, l, B)
        self.dump(f"x1{kind}{l}_{t0}", B["xT"].v)
        if "nopeer" not in FLAGS:
            self.peer(kind, t0, TT, l, B)
        self.dump(f"x2{kind}{l}_{t0}", B["xT"].v)

    def mixer(self, kind, t0, TT, l, B):
        P = kind == "p"
        subt = [(i * 128, 128) for i in range(4)] if P else [(0, TS)]
        hT = B["hT"]
        self.norm_mod(kind, B, TT, l, 1)
        self.dump(f"h{kind}{l}_{t0}", hT.v, BF16)
        self.dma(B["wal1"].v, self.wal_in[l], eng="gpsimd")
        self.memset(B["glrT1"].v, 1.0)
        s = self.slot()
        self.wload(s, self.w_in, l, OFF_GLR, 16)
        self.proj_fm(s, 16, hT, TT, lambda cc, ps: self.cp(B["glrT1"][0:16, :], ps, eng="scalar"))
        for g in range(2):
            s = self.slot()
            self.wload(s, self.w_in, l, OFF_GQ + g * 512, 512)
            self.proj_fm(s, 512, hT, TT, lambda cc, ps, g=g: self.cp(B["qT"][:, g * 4 + cc, :], ps, eng="scalar"))
        for g in range(2):
            s = self.slot()
            self.wload(s, self.w_in, l, OFF_GK + g * 512, 512)
            self.proj_fm(s, 512, hT, TT, lambda cc, ps, g=g: self.cp(B["kT"][:, g * 4 + cc, :], ps, eng="vector"))
            self.proj_tm(s, 512, hT, subt,
                         lambda si, ps, g=g: self.cp(B["ktok"][:, si, g * 512:(g + 1) * 512], ps, eng="scalar"))
        for g in range(4):
            s = self.slot()
            self.wload(s, self.w_in, l, OFF_GV + g * 512, 512)
            self.proj_tm(s, 512, hT, subt,
                         lambda si, ps, g=g: self.cp(B["vtok"][:, si, g * 512:(g + 1) * 512], ps,
                                                     eng="vector" if si % 2 else "scalar"))
        for ci, (c0, C) in enumerate(subt):
            if "nogla" not in FLAGS:
                self.gla_chunk(kind, l, B, ci, c0, C, t0)
        if P and t0 + TT == SEQ:
            dst = self.glap_out.t[l].rearrange("h (dc p) e -> p (h dc) e", p=128)
            self.dma(V(self.glap_out, dst), self.S[l].v)
        if "noswa" in FLAGS:
            pass
        elif P:
            self.swa_prompt(l, B, t0, TT)
        else:
            self.swa_sample(l, B)
        for (off, fn, mode) in ((OFF_GR, AF.Silu, 0), (OFF_GA, AF.Sigmoid, 0), (OFF_GB, AF.Sigmoid, 1)):
            for g in range(4):
                s = self.slot()
                self.wload(s, self.w_in, l, off + g * 512, 512)

                def c_gate(cc, ps, g=g, fn=fn, mode=mode):
                    ch = g * 4 + cc
                    tmp = B["gtmp"][cc % 2]
                    self.act(tmp.v, ps, fn)
                    if mode == 0:
                        self.tt(B["onT"][:, ch, :], B["onT"][:, ch, :], tmp.v, ALU.mult)
                    else:
                        self.tt(tmp.v, tmp.v, B["oswaT"][:, ch, :], ALU.mult)
                        self.tt(B["onT"][:, ch, :], B["onT"][:, ch, :], tmp.v, ALU.add)
                self.proj_fm(s, 512, hT, TT, c_gate)
        if not P:
            self.dma(B["mG"].v, self.load_mod_s(B, l, 2))
        for g in range(4):
            s = self.slot()
            self.wload(s, self.w_out, l, g * 512, 512)
            self.proj_fm(s, 512, B["onT"], TT, lambda cc, ps, g=g: self.residual(kind, B, l, 1, g * 4 + cc, ps))

    def gla_chunk(self, kind, l, B, ci, c0, C, t0):
        P = kind == "p"
        nseq = 1 if P else NSEQ_S
        tri = self.cfv("trip") if P else self.cfv("tris", 64)
        dmat = self.cfv("dp") if P else self.cfv("ds", 64)
        mt = self.cfv("mtp") if P else self.cfv("mts", 64)
        ssel = self.cfv("sselp") if P else self.cfv("ssels", 64)
        first = P and t0 == 0 and ci == 0
        L_ = B["l"]
        eb, enb = B["eb"], B["enb"]
        for hf in range(2):
            ps = self.bank()
            self.mm(ps[0:C, :], B["glrT1"][:, c0:c0 + C], B["wal1"][:, hf * 512:(hf + 1) * 512])
            self.act(L_[:, hf * 512:(hf + 1) * 512], ps[0:C, :], AF.Exp, scale=-1.0)
        self.act(L_.v, L_.v, AF.Ln, bias=self.smallf[0:C, 1:2])
        psb = [self.bank(), self.bank()]
        for dc in range(8):
            self.mm(psb[dc // 4][:, (dc % 4) * C:(dc % 4 + 1) * C], L_[:, dc * 128:(dc + 1) * 128], tri)
        for hf in range(2):
            pv_ = psb[hf][:, 0:4 * C].re("p (a b) -> p a b", a=4)
            self.act(eb[:, hf * 4:(hf + 1) * 4, :], pv_, AF.Exp)
            self.act(enb[:, hf * 4:(hf + 1) * 4, :], pv_, AF.Exp, scale=-1.0)
        self.stt(eb.v, eb.v, 0.0625, B["qT"][:, :, c0:c0 + C], ALU.mult, ALU.mult)
        self.tt(enb.v, enb.v, B["kT"][:, :, c0:c0 + C], ALU.mult)
        pse = self.bank()
        for dc in range(8):
            self.mm(pse[:, dc * nseq:(dc + 1) * nseq], L_[:, dc * 128:(dc + 1) * 128], ssel)
        self.act(B["ebend"][:, :, 0:nseq], pse[:, 0:8 * nseq].re("p (a b) -> p a b", a=8), AF.Exp)
        pq = [self.bank(), self.bank()]
        for hf in range(2):
            self.mm(pq[hf][0:C, :], dmat, L_[:, hf * 512:(hf + 1) * 512])
        for hf in range(2):
            self.act(B["ebe"][:, hf * 512:(hf + 1) * 512], pq[hf][0:C, :], AF.Exp)
        self.tt(B["khat"].v, B["ktok"][:, ci, :], B["ebe"].v, ALU.mult)
        psa = self.bank()
        for h in range(4):
            for dc in range(2):
                self.mm(psa[0:C, h * C:(h + 1) * C], enb[:, 2 * h + dc, :], eb[:, 2 * h + dc, :],
                        start=(dc == 0), stop=(dc == 1))
        self.tt(B["attT"].v, psa[0:C, 0:4 * C].re("p (h c) -> p h c", h=4), mt.unsq(1).bc([C, 4, C]), ALU.mult)
        for h in range(4):
            if P:
                pso = self.bank()
                oviews = [pso[:, ec * C:(ec + 1) * C] for ec in range(4)]
                for ec in range(4):
                    self.mm(oviews[ec], B["vtok"][:, ci, h * 512 + ec * 128:h * 512 + (ec + 1) * 128],
                            B["attT"][:, h, :], start=True, stop=first)
                    if not first:
                        for dc in range(2):
                            self.mm(oviews[ec], self.S[l][:, 2 * h + dc, ec * 128:(ec + 1) * 128],
                                    eb[:, 2 * h + dc, :], start=False, stop=(dc == 1))
                excl = set()
            else:
                oviews = [self.banks[ec][:, 0:C] for ec in range(4)]
                excl = {0, 1, 2, 3}
                for ec in range(4):
                    self.mm(oviews[ec], B["vtok"][:, 0, h * 512 + ec * 128:h * 512 + (ec + 1) * 128],
                            B["attT"][:, h, :], start=True, stop=False)
                for b in range(NSEQ_S):
                    S0 = B["S0"][b % 2]
                    self.dma(S0.v, V(self.state_in, self.state_in.t[l, b, h].rearrange("(dc p) e -> p dc e", p=128)))
                    for ec in range(4):
                        for dc in range(2):
                            self.mm(self.banks[ec][:, LS * b:LS * b + LS], S0[:, dc, ec * 128:(ec + 1) * 128],
                                    eb[:, 2 * h + dc, LS * b:LS * b + LS], start=False,
                                    stop=(b == NSEQ_S - 1 and dc == 1))
                    khm = B["khm"][b % 2]
                    self.ts(khm[:, 0:256], B["khat"][:, h * 256:(h + 1) * 256],
                            self.cfv("ssel01", 64)[:, b:b + 1], ALU.mult)
                    for dc in range(2):
                        pss = self.bank_ex(excl)
                        self.mm(pss[:, :], khm[:, dc * 128:(dc + 1) * 128], B["vtok"][:, 0, h * 512:(h + 1) * 512])
                        sn = B["Snew"][dc]
                        self.stt(sn.v, S0[:, dc, :], B["ebend"][:, 2 * h + dc, b:b + 1], pss[:, :], ALU.mult, ALU.add)
                        self.dma(V(self.glas_out, self.glas_out.t[l, b, h, dc * 128:(dc + 1) * 128, :]), sn.v)
            osq = B["osq"]
            for ec in range(4):
                self.act(osq[:, ec, 0:C], oviews[ec], AF.Square)
            psn = self.bank_ex(excl)
            for ec in range(4):
                self.mm(psn[:, 0:C], self.cfv("ones"), osq[:, ec, 0:C], start=(ec == 0), stop=(ec == 3))
            ors = B["orstd"][:, 0:C]
            self.act(ors, psn[:, 0:C], AF.Ln, scale=1.0 / 512.0, bias=self.smallf[:, 0:1])
            self.act(ors, ors, AF.Exp, scale=-0.5)
            for ec in range(4):
                self.stt(B["onT"][:, 4 * h + ec, c0:c0 + C], oviews[ec],
                         self.pp[l][:, PP_GNG + ec:PP_GNG + ec + 1], ors, ALU.mult, ALU.mult)
            if P:
                for dc in range(2):
                    idx = 2 * h + dc
                    pss = self.bank()
                    self.mm(pss[:, :], B["khat"][:, idx * 128:(idx + 1) * 128], B["vtok"][:, ci, h * 512:(h + 1) * 512])
                    if first:
                        self.cp(self.S[l][:, idx, :], pss[:, :], eng="scalar")
                    else:
                        self.stt(self.S[l][:, idx, :], self.S[l][:, idx, :], B["ebend"][:, idx, 0:1], pss[:, :],
                                 ALU.mult, ALU.add)

    def swa_proj(self, l, B, TT, subt, c_kv):
        hT = B["hT"]
        if "sp_nosq" not in FLAGS:
            for g in range(4):
                s = self.slot()
                self.wload(s, self.w_in, l, OFF_SQ + g * 512, 512)
                self.proj_fm(s, 512, hT, TT, lambda cc, ps, g=g: self.cp(B["sqT"][:, g * 4 + cc, :], ps,
                                                                         eng="scalar" if cc % 2 else "vector"))
        s = self.slot()
        self.wload(s, self.w_in, l, OFF_SK, 512)
        if "sp_notm" not in FLAGS:
            self.proj_tm(s, 512, hT, subt, c_kv)
        if "sp_nodup" in FLAGS:
            return
        s2 = self.slot()
        for kv in range(4):
            for dup in range(2):
                self.cp(s2[:, :, kv * 128 + dup * 64:kv * 128 + dup * 64 + 64], s[:, :, kv * 64:(kv + 1) * 64],
                        eng="vector")
        self.proj_fm(s2, 512, hT, TT, lambda cc, ps: self.cp(B["skT"][:, cc, 128:128 + TT], ps, eng="scalar"))

    def swa_prompt(self, l, B, t0, TT):
        first_tile = t0 == 0
        last_tile = t0 + TT == SEQ
        subt = [(i * 128, 128) for i in range(4)]
        if not first_tile:
            self.cp(B["skT"][:, :, 0:128], self.pskT[l].v, eng="gpsimd")
            self.cp(B["sv1"][:, 0, :], self.psv[l].v, eng="gpsimd")

        def c_kv(si, ps):
            self.cp(B["sv1"][:, si + 1, :], ps[:, 256:512], eng="vector")
            if last_tile and si == 3:
                self.cp(B["kvo"].v, ps, eng="scalar")
                self.dma(self.kp_out[l], B["kvo"][:, 0:256])
                self.dma(self.vp_out[l], B["kvo"][:, 256:512])
        self.swa_proj(l, B, TT, subt, c_kv)
        if not last_tile:
            self.cp(self.pskT[l].v, B["skT"][:, :, TT:TT + 128], eng="gpsimd")
            self.cp(self.psv[l].v, B["sv1"][:, 4, :], eng="gpsimd")
        numb, denb = self.banks[0], self.banks[1]
        excl = {0, 1}
        onesb = self.cbv("ones")[:, 0:64]
        swam = self.cbv("swam")
        it = 0
        for chunk in range(16):
            kv = chunk // 4
            for half in range(2):
                pr = slice(64 * half, 64 * half + 64)
                for qb in range(4):
                    has_prev = not (first_tile and qb == 0)
                    ps = self.bank_ex(excl)
                    q = B["sqT"][pr, chunk, qb * 128:(qb + 1) * 128]
                    if has_prev:
                        self.mm(ps[:, 0:128], B["skT"][pr, kv, qb * 128:(qb + 1) * 128], q)
                    self.mm(ps[:, 128:256], B["skT"][pr, kv, (qb + 1) * 128:(qb + 2) * 128], q)
                    PT = B["PT"][it % 2]
                    it += 1
                    lo = 0 if has_prev else 128
                    self.act(PT[:, lo:256], ps[:, lo:256], AF.Exp, scale=0.125)
                    self.tt(PT[:, lo:256], PT[:, lo:256], swam[:, lo:256], ALU.mult, eng="gpsimd")
                    n_ = numb[pr, qb * 128:(qb + 1) * 128]
                    d_ = denb[pr, qb * 128:(qb + 1) * 128]
                    v_prev = B["sv1"][:, qb, kv * 64:(kv + 1) * 64]
                    v_cur = B["sv1"][:, qb + 1, kv * 64:(kv + 1) * 64]
                    if has_prev:
                        self.mm(n_, v_prev, PT[:, 0:128], start=True, stop=False)
                        self.mm(n_, v_cur, PT[:, 128:256], start=False, stop=True)
                        self.mm(d_, onesb, PT[:, 0:128], start=True, stop=False)
                        self.mm(d_, onesb, PT[:, 128:256], start=False, stop=True)
                    else:
                        self.mm(n_, v_cur, PT[:, 128:256])
                        self.mm(d_, onesb, PT[:, 128:256])
            g0 = B["gtmp"][chunk % 2]
            self.ts(g0.v, denb[:, 0:TT], self.esink[l][:, chunk:chunk + 1], ALU.add)
            self.recip(g0.v, g0.v)
            self.tt(B["oswaT"][:, chunk, :], numb[:, 0:TT], g0.v, ALU.mult)

    def swa_sample(self, l, B):
        TT = TS
        subt = [(0, TS)]

        def c_kv(si, ps):
            self.cp(B["sv1"][:, 1, :], ps[:, 256:512], eng="vector")
            self.cp(B["knew"].v, ps, eng="scalar")
        if "swa_noproj" not in FLAGS:
            self.swa_proj(l, B, TT, subt, c_kv)
        cksrc = self.ckT_in[l][:, :, 0:2048]
        if "swa_nocache" not in FLAGS:
            self.dma(B["ckT"][0:64].re("p (a c) b -> p a (c b)", c=16), cksrc, eng="gpsimd")
            self.dma(B["ckT"][64:128].re("p (a c) b -> p a (c b)", c=16), cksrc, eng="gpsimd")
        if "swa_nocv" not in FLAGS:
            self.dma(B["cv"].v, V(self.cv_in, self.cv_in.t[l].rearrange("b k c -> k b c")), eng="gpsimd")
        if "swa_nodd" not in FLAGS:
            self.dma(self.ks_out[l][:, 0:124, :], self.ck_in[l][:, 4:128, :])
            self.dma(self.vs_out[l][:, 0:124, :], self.cv_in[l][:, 4:128, :])
        for b in range(NSEQ_S if "swa_noout" not in FLAGS else 0):
            self.dma(self.ks_out[l][b, 124:128, :], B["knew"][LS * b:LS * b + LS, 0:256])
            self.dma(self.vs_out[l][b, 124:128, :], B["knew"][LS * b:LS * b + LS, 256:512])
        onesb = self.cbv("ones")[:, 0:64]
        it = 0
        for b in range(NSEQ_S if "swa_nobody" not in FLAGS else 0):
            for kv in range(4):
                pss = self.bank()
                psn_ = self.bank()
                for j in range(8):
                    hq = 8 * kv + j
                    chunk, half = hq // 2, hq % 2
                    pr = slice(64 * half, 64 * half + 64)
                    q = B["sqT"][pr, chunk, LS * b:LS * b + LS]
                    self.mm(pss[:, 4 * j:4 * j + 4], B["ckT"][pr, b * 4 + kv, :], q)
                    self.mm(psn_[0:TS, 4 * j:4 * j + 4], B["skT"][pr, kv, 128:128 + TS], q)
                PTs = B["PTs"][it % 2]
                PTn = B["PTn"][it % 2]
                it += 1
                self.act(PTs.v, pss[:, 0:32], AF.Exp, scale=0.125)
                self.tt(PTs.v, PTs.v, self.cbv("pastm"), ALU.mult, eng="gpsimd")
                self.act(PTn.v, psn_[0:TS, 0:32], AF.Exp, scale=0.125)
                self.tt(PTn.v, PTn.v, self.newm[:, b, :], ALU.mult, eng="gpsimd")
                nb = self.bank()
                db = self.bank()
                for half in range(2):
                    pr = slice(64 * half, 64 * half + 64)
                    pc = PTs.v.re("p (j i) -> p j i", i=4)[:, half::2, :]
                    nc_ = PTn.v.re("p (j i) -> p j i", i=4)[:, half::2, :]
                    n_ = nb[pr, 0:16].re("p (a b) -> p a b", b=4)
                    d_ = db[pr, 0:16].re("p (a b) -> p a b", b=4)
                    self.mm(n_, B["cv"][:, b, kv * 64:(kv + 1) * 64], pc, start=True, stop=False)
                    self.mm(n_, B["sv1"][:, 1, kv * 64:(kv + 1) * 64], nc_, start=False, stop=True)
                    self.mm(d_, onesb, pc, start=True, stop=False)
                    self.mm(d_, onesb[0:TS], nc_, start=False, stop=True)
                r3 = B["rden"][it % 2][:, 0:16].re("p (a b) -> p a b", b=4)
                self.tt(r3, db[:, 0:16].re("p (a b) -> p a b", b=4),
                        self.esink[l][:, 4 * kv:4 * kv + 4].unsq(2).bc([128, 4, 4]), ALU.add)
                self.recip(r3, r3)
                self.tt(B["oswaT"][:, 4 * kv:4 * kv + 4, LS * b:LS * b + LS],
                        nb[:, 0:16].re("p (a b) -> p a b", b=4), r3, ALU.mult)

    def peer(self, kind, t0, TT, l, B):
        P = kind == "p"
        subt = [(i * 128, 128) for i in range(4)] if P else [(0, TS)]
        self.norm_mod(kind, B, TT, l, 2)
        hT = B["hT"]
        if not P:
            self.dma(B["mG"].v, self.load_mod_s(B, l, 5))
        for g in range(4):
            s = self.slot()
            self.wload(s, self.peer_wq, l, g * 512, 512)
            self.proj_fm(s, 512, hT, TT, lambda cc, ps, g=g: self.cp(B["pqT"][:, g * 4 + cc, :], ps,
                                                                     eng="scalar" if cc % 2 else "vector"))
        self.dma(B["pkT"].v, self.pkT_in[l], eng="gpsimd")
        self.memset(B["thT"].v, 0.0, eng="gpsimd")
        ident = self.cfv("ident")
        pm = B["pm"]
        pmb = B["pmb"]
        for si, (c0, n) in enumerate(subt):
            sc = B["sc"]
            for half in range(2):
                ps2 = [self.bank(), self.bank()]
                for q in range(8):
                    hc = half * 8 + q
                    self.mm(ps2[q // 4][0:n, (q % 4) * 128:(q % 4 + 1) * 128], B["pqT"][:, hc, c0:c0 + n],
                            B["pkT"][:, hc, :])
                for j in range(2):
                    self.cp(sc[:, half * 8 + j * 4:half * 8 + j * 4 + 4, :],
                            ps2[j][0:n, :].re("p (a b) -> p a b", a=4), eng="scalar")
            top16 = B["top16"]
            for hc in range(16):
                self.max8(top16[:, hc, 0:8], sc[:, hc, :])
                self.mrep(B["scr"].v, top16[:, hc, 0:8], sc[:, hc, :])
                self.max8(top16[:, hc, 8:16], B["scr"].v)
            c24 = B["c24"]
            cm = B["comb"]
            for h in range(8):
                self.tt(cm[0].v.re("p (a b) -> p a b", a=16), top16[:, 2 * h, :].unsq(2).bc([n, 16, 16]),
                        top16[:, 2 * h + 1, :].unsq(1).bc([n, 16, 16]), ALU.add)
                self.max8(c24[:, h, 0:8], cm[0].v)
                self.mrep(cm[1].v, c24[:, h, 0:8], cm[0].v)
                self.max8(c24[:, h, 8:16], cm[1].v)
                self.mrep(cm[2].v, c24[:, h, 8:16], cm[1].v)
                self.max8(c24[:, h, 16:24], cm[2].v)
            thr, negm, Z, gf = pm[:, 0:8], pm[:, 8:16], pm[:, 16:24], pm[:, 24:32]
            r0, r1, r2 = pm[:, 32:40], pm[:, 40:48], pm[:, 48:56]
            ex = pm[:, 64:192]
            th3 = pm[:, 192:216]
            th3v = th3.re("p (h k) -> p h k", k=3)
            self.tt(thr, c24[:, :, 15], c24[:, :, 16], ALU.add)
            self.ts(thr, thr, 0.5, ALU.mult)
            self.ts(negm, c24[:, :, 0], -1.0, ALU.mult)
            for h in range(8):
                self.act(ex[:, h * 16:(h + 1) * 16], c24[:, h, 0:16], AF.Exp, bias=negm[:, h:h + 1],
                         accum=Z[:, h:h + 1])
            self.tt(gf, thr, negm, ALU.add)
            self.act(gf, gf, AF.Exp)
            self.recip(Z, Z)
            self.tt(gf, gf, Z, ALU.mult)
            self.ts(r0, thr, -1.0, ALU.mult)
            self.cp(pmb[:, 0:8], r0)
            self.tt(r1, r0, pmb[:, 0:8], ALU.subtract)
            self.cp(pmb[:, 8:16], r1)
            self.tt(r2, r1, pmb[:, 8:16], ALU.subtract)
            self.cp(pmb[:, 16:24], r2)
            for kk in range(3):
                self.cp(th3v[:, :, kk], pmb[:, 8 * kk:8 * kk + 8])
            pst = self.bank()
            self.transpose(pst[0:24, 0:n], th3, ident[0:n, 0:n])
            self.cp(B["thT"][0:24, c0:c0 + n], pst[0:24, 0:n], eng="scalar")
            pst2 = self.bank()
            self.transpose(pst2[0:8, 0:n], gf, ident[0:n, 0:n])
            self.cp(B["gfT"][:, c0:c0 + n], pst2[0:8, 0:n], eng="scalar")
        for h in range(8):
            ps = self.bank()
            self.mm(ps[:, 0:TT], ident[0:8, h:h + 1].bc([8, 128]), B["gfT"][:, 0:TT])
            self.cp(B["gbcb"][:, h, :], ps[:, 0:TT], eng="scalar")
        sel24 = self.cbv("sel24")
        for blk in range(32):
            su = self.slot()
            self.wload(su, self.uT, l, blk * 512, 512)
            sv = self.slot()
            svv = sv.v.re("p a b -> p (a b)").re("p (a b) -> p a b", a=4)
            vsrc = V(self.pv, self.pv.t[l, blk * 512:(blk + 1) * 512, :].rearrange("(nc p) d -> p nc d", p=128))
            self.dma(svv, vsrc, eng="gpsimd")
            AT = B["AT"][blk % 2]
            hraw = B["hraw"]
            for nc_ in range(4):
                ps = self.bank()
                for kc in range(16):
                    self.mm(ps[:, 0:TT], su[:, kc, nc_ * 128:(nc_ + 1) * 128], hT[:, kc, 0:TT],
                            start=(kc == 0), stop=(kc == 15))
                self.cp(hraw[:, nc_, :], ps[:, 0:TT], eng="scalar")
            self.act(hraw.v, hraw.v, AF.Gelu)
            for pair in range(2):
                ncs = (2 * pair, 2 * pair + 1)
                for h in range(8):
                    pzs = []
                    for q, nc_ in enumerate(ncs):
                        j = blk * 4 + nc_
                        pz = self.bank()
                        self.mm(pz[:, 0:TT], B["pkT"][:, 2 * h + 1, :], B["pqT"][:, 2 * h + 1, :], start=True, stop=False)
                        self.mm(pz[:, 0:TT], B["pkT"][:, 2 * h, j:j + 1].bc([128, 128]), B["pqT"][:, 2 * h, :],
                                start=False, stop=False)
                        self.mm(pz[:, 0:TT], sel24[:, h:h + 1].bc([128, 128]), B["thT"][:, 0:TT], start=False, stop=True)
                        pzs.append(pz)
                    for q in range(2):
                        self.act(B["zc"][q].v, pzs[q][:, 0:TT], AF.Prelu, alpha=1.0e7)
                    e0 = (h % 2) * 2
                    for q in range(2):
                        self.act(B["E"][e0 + q].v, B["zc"][q].v, AF.Exp)
                    if h == 0:
                        for q in range(2):
                            self.tt(B["acc"][q].v, B["E"][e0 + q].v, B["gbcb"][:, 0, :], ALU.mult)
                    else:
                        for q in range(2):
                            self.tt(B["ptmp"][e0 + q].v, B["E"][e0 + q].v, B["gbcb"][:, h, :], ALU.mult)
                        for q in range(2):
                            self.tt(B["acc"][q].v, B["acc"][q].v, B["ptmp"][e0 + q].v, ALU.add)
                for q, nc_ in enumerate(ncs):
                    self.tt(AT[:, nc_, :], hraw[:, nc_, :], B["acc"][q].v, ALU.mult)
            for dch in range(16):
                po = self.bank()
                for nc_ in range(4):
                    self.mm(po[:, 0:TT], svv[:, nc_, dch * 128:(dch + 1) * 128], AT[:, nc_, :],
                            start=(nc_ == 0), stop=(nc_ == 3))
                self.residual(kind, B, l, 2, dch, po[:, 0:TT])


_PROG = None


def _prep_inputs(inp):
    f = lambda a: np.ascontiguousarray(np.asarray(a, dtype=np.float32))
    cf, cb = make_consts()
    L = DEPTH
    shared = {
        "w_ada": f(inp["w_ada"]), "w_in": f(inp["w_in"]), "w_out": f(inp["w_out"]), "peer_wq": f(inp["peer_wq"]),
        "uT": f(np.transpose(np.asarray(inp["peer_u"]), (0, 2, 1))),
        "pv": f(inp["peer_v"]),
        "pkT": f(np.transpose(np.asarray(inp["peer_keys"]).reshape(L, 16, 128, 128), (0, 3, 1, 2))),
        "fg": f(np.asarray(inp["final_g"]).reshape(16, 128).T),
        "wal1": f(np.concatenate([np.asarray(inp["w_alpha2"]), np.asarray(inp["b_alpha"])[:, None, :]], axis=1)),
        "cf": cf, "cb": cb,
    }
    pp = np.zeros((L, 128, NPP), np.float32)
    for l in range(L):
        pp[l, :, PP_G1:PP_G1 + 16] = np.asarray(inp["norm1_g"])[l].reshape(16, 128).T
        pp[l, :, PP_G2:PP_G2 + 16] = np.asarray(inp["norm2_g"])[l].reshape(16, 128).T
        pp[l, :, PP_BADA:PP_BADA + 96] = np.asarray(inp["b_ada"])[l].reshape(96, 128).T
        pp[l, :, PP_GNG:PP_GNG + 4] = np.asarray(inp["gla_norm_g"])[l].reshape(4, 128).T
        sk = np.asarray(inp["swa_sinks"])[l]
        pp[l, :, PP_SINK:PP_SINK + 16] = np.repeat(sk.reshape(16, 2).T, 64, axis=0)
    shared["pp"] = pp
    xp = np.asarray(inp["x_prompt"])
    xs = np.asarray(inp["x_sample"])
    cp_ = np.asarray(inp["c_prompt"])
    cs = np.asarray(inp["c_sample"])
    st = np.asarray(inp["state_gla"])
    ck = np.asarray(inp["cache_swa_k"])
    cv = np.asarray(inp["cache_swa_v"])
    maps = []
    for i in range(NCORES):
        sl = slice(NSEQ_S * i, NSEQ_S * (i + 1))
        m = dict(shared)
        m["xT"] = f(np.concatenate([xp[i].T, xs[sl].reshape(TS, D).T], axis=1))
        m["cT"] = f(np.concatenate([cp_[i][None, :], np.repeat(cs[sl], LS, axis=0)], axis=0).T)
        m["state"] = f(st[:, sl])
        cki = ck[:, sl]
        ckt = np.zeros((L, 64, 4, 2048 + 64), np.float32)
        ckt[:, :, :, :2048] = np.transpose(cki, (0, 4, 1, 3, 2)).reshape(L, 64, 4, 2048)
        m["ckT"] = ckt
        m["ck"] = f(cki.reshape(L, NSEQ_S, 128, 256))
        m["cv"] = f(cv[:, sl].reshape(L, NSEQ_S, 128, 256))
        maps.append(m)
    return maps


def _assemble(res):
    L = DEPTH
    yp = np.stack([res[i]["yT"][:, :SEQ].T for i in range(NCORES)])
    ys = np.concatenate([res[i]["yT"][:, SEQ:].T.reshape(NSEQ_S, LS, D) for i in range(NCORES)])
    glap = np.stack([res[i]["gla_p"] for i in range(NCORES)], axis=1)
    kp = np.stack([res[i]["k_p"].reshape(L, 128, 4, 64) for i in range(NCORES)], axis=1)
    vp = np.stack([res[i]["v_p"].reshape(L, 128, 4, 64) for i in range(NCORES)], axis=1)
    glas = np.concatenate([res[i]["gla_s"] for i in range(NCORES)], axis=1)
    ks = np.concatenate([res[i]["k_s"].reshape(L, NSEQ_S, 128, 4, 64) for i in range(NCORES)], axis=1)
    vs = np.concatenate([res[i]["v_s"].reshape(L, NSEQ_S, 128, 4, 64) for i in range(NCORES)], axis=1)
    return tuple(np.ascontiguousarray(a, dtype=np.float32) for a in (yp, ys, glap, kp, vp, glas, ks, vs))


def kernel(**inputs):
    global _PROG
    if _PROG is None:
        _PROG = Prog()
    maps = _prep_inputs(inputs)
    r = run_bass_kernel_spmd(_PROG.nc, maps, core_ids=list(range(NCORES)))
    return _assemble(r.results)
```

```python
import numpy as np
import concourse.bass as bass
import concourse.mybir as mybir
from concourse.bass_utils import run_bass_kernel_spmd

F32 = mybir.dt.float32
BF16 = mybir.dt.bfloat16
U8 = mybir.dt.uint8
AF = mybir.ActivationFunctionType
ALU = mybir.AluOpType

NCORES = 8
D = 2048
DEPTH = 2
SEQ = 2048
NSEQ_S = 16
LS = 4
TS = NSEQ_S * LS
TTOK = SEQ + TS
INW = 12816
OFF_GQ, OFF_GK, OFF_GV, OFF_GR, OFF_GLR, OFF_SQ, OFF_SK, OFF_SV, OFF_GA, OFF_GB = (
    0, 1024, 2048, 4096, 6144, 6160, 8208, 8464, 8720, 10768)
EPS = 1e-6
EPOCH = 24000
NEG = -1.0e30
import os as _os
FLAGS = set(_os.environ.get("MKFLAGS", "").split(","))


class V:
    __slots__ = ("tile", "ap")

    def __init__(self, tile, ap):
        self.tile = tile
        self.ap = ap

    def __getitem__(self, idx):
        return V(self.tile, self.ap[idx])

    def bc(self, shape):
        return V(self.tile, self.ap.to_broadcast(list(shape)))

    def re(self, s, **kw):
        return V(self.tile, self.ap.rearrange(s, **kw))

    def unsq(self, ax):
        return V(self.tile, self.ap.unsqueeze(ax))

    @property
    def shape(self):
        return tuple(self.ap.shape)


class T:
    def __init__(self, t, name, space, rng=None):
        self.t = t
        self.name = name
        self.space = space
        self.last_w = None
        self.reads = []
        self.dsem = None
        self.dval = 0
        self.ro = False
        self.wo = False
        self.rng = rng
        self.aliases = []

    def __getitem__(self, idx):
        return V(self, self.t[idx])

    @property
    def v(self):
        return V(self, self.t)


class EngS:
    def __init__(self, name):
        self.name = name
        self.prog = []
        self.sems = []
        self.cnt = 0
        self.seen = {}


class K:
    def __init__(self, nc):
        self.nc = nc
        self.E = {n: EngS(n) for n in ("tensor", "vector", "scalar", "gpsimd", "sync")}
        self.nsem = 0
        self.ntile = 0
        self.dma_tiles = []
        self.swq = []
        self.arena = None
        self.carved = []

    def _sem(self, nm):
        self.nsem += 1
        return self.nc.alloc_semaphore(f"s{self.nsem}_{nm}"[:40])

    def make_arena(self, nbytes):
        self.arena = self.nc.alloc_sbuf_tensor("arena", [128, nbytes], U8)
        self.arena_bytes = nbytes

    def carve(self, off, shape, dt, name):
        isz = 4 if dt == F32 else (2 if dt == BF16 else 1)
        n = 1
        for s in shape[1:]:
            n *= s
        nb = n * isz
        assert off % 4 == 0 and off + nb <= self.arena_bytes, (name, off, nb, self.arena_bytes)
        ap = self.arena[0:shape[0], off:off + nb].bitcast(dt)
        if len(shape) == 3:
            ap = ap.rearrange("p (a b) -> p a b", a=shape[1])
        elif len(shape) == 4:
            ap = ap.rearrange("p (a b c) -> p a b c", a=shape[1], b=shape[2])
        self.ntile += 1
        t = T(ap, f"{name}_{self.ntile}", "sb", (off, off + nb))
        for o in self.carved:
            if o.rng[0] < t.rng[1] and t.rng[0] < o.rng[1]:
                o.aliases.append(t)
                t.aliases.append(o)
        self.carved.append(t)
        return t

    def ps(self, name):
        self.ntile += 1
        h = self.nc.alloc_psum_tensor(f"{name}_{self.ntile}", [128, 512], F32)
        return T(h[:, :], name, "ps")

    def dram(self, name, shape, dt, kind):
        h = self.nc.dram_tensor(name, list(shape), dt, kind=kind)
        t = T(h.ap(), name, "dram")
        if kind == "ExternalInput":
            t.ro = True
        if kind == "ExternalOutput":
            t.wo = True
        return t

    def _need(self, E, sem, val):
        key = id(sem)
        if val <= 0 or E.seen.get(key, 0) >= val:
            return
        E.seen[key] = val
        E.prog.append(("wait", sem, val))

    def _deps(self, E, reads, writes):
        for t0 in reads:
            if t0.ro:
                continue
            for t in [t0] + t0.aliases:
                if t.last_w is not None:
                    sem, val, en = t.last_w
                    if not (en == E.name and en == "tensor"):
                        self._need(E, sem, val)
                if t.space == "ps":
                    for (sem, val, en) in t.reads:
                        if en != E.name:
                            self._need(E, sem, val)
        for t0 in writes:
            for t in [t0] + t0.aliases:
                if t.last_w is not None:
                    sem, val, en = t.last_w
                    if en != E.name:
                        self._need(E, sem, val)
                for (sem, val, en) in t.reads:
                    if en != E.name:
                        self._need(E, sem, val)

    def op(self, eng, fn, reads=(), writes=()):
        E = self.E[eng]
        self._deps(E, reads, writes)
        if not E.sems or E.cnt >= EPOCH:
            E.sems.append(self._sem(eng))
            E.cnt = 0
        E.cnt += 1
        sem = E.sems[-1]
        E.prog.append(("op", fn, sem))
        rec = (sem, E.cnt, eng)
        for t in reads:
            if t.ro:
                continue
            t.reads.append(rec)
            if len(t.reads) > 48:
                d = {}
                for r in t.reads:
                    kk = id(r[0])
                    if kk not in d or d[kk][1] < r[1]:
                        d[kk] = r
                t.reads = list(d.values())
        for t in writes:
            t.last_w = rec
            t.reads = []

    def dma(self, eng, out, in_):
        E = self.E[eng]
        out_t, in_t = out.tile, in_.tile
        if eng == "gpsimd":
            def ndesc(ap):
                n = 1
                for d in ap.shape[:-1]:
                    n *= d
                return n
            nd = (max(ndesc(out.ap), ndesc(in_.ap)) + 15) // 16 + 2
            fifo = self.swq
            while fifo and sum(x[2] for x in fifo) + nd > 640:
                sem0, val0, _ = fifo.pop(0)
                self._need(E, sem0, val0)
        self._deps(E, [in_t], [out_t])
        sb_t = out_t if out_t.space != "dram" else in_t
        if sb_t.dsem is None:
            sb_t.dsem = self._sem("d" + sb_t.name)
            self.dma_tiles.append(sb_t)
        sb_t.dval += 16
        sem, val = sb_t.dsem, sb_t.dval
        E.prog.append(("dma", out.ap, in_.ap, sem))
        if eng == "gpsimd":
            self.swq.append((sem, val, nd))
        if not out_t.ro and not out_t.wo:
            out_t.last_w = (sem, val, "dma")
            out_t.reads = []
        if not in_t.ro:
            in_t.reads.append((sem, val, "dma"))

    def emit(self):
        nc = self.nc
        S = self.E["sync"]
        for t in self.dma_tiles:
            self._need(S, t.dsem, t.dval)
        with nc.Block() as block:
            def run(E, eng):
                for ent in E.prog:
                    if ent[0] == "wait":
                        eng.wait_ge(ent[1], ent[2])
                    elif ent[0] == "op":
                        ent[1](eng).then_inc(ent[2], 1)
                    else:
                        eng.dma_start(out=ent[1], in_=ent[2]).then_inc(ent[3], 16)

            @block.tensor
            def _(e):
                run(self.E["tensor"], e)

            @block.vector
            def _(e):
                run(self.E["vector"], e)

            @block.scalar
            def _(e):
                run(self.E["scalar"], e)

            @block.gpsimd
            def _(e):
                run(self.E["gpsimd"], e)

            @block.sync
            def _(e):
                run(self.E["sync"], e)

    def stats(self):
        return ({n: sum(1 for x in e.prog if x[0] != "wait") for n, e in self.E.items()},
                {n: sum(1 for x in e.prog if x[0] == "wait") for n, e in self.E.items()}, self.nsem)


CF = {}
_o = 0
for _n, _w in [("ident", 128), ("ones", 128), ("trip", 128), ("dp", 128), ("mtp", 128),
               ("tris", 64), ("ds", 64), ("mts", 64), ("ssels", 16), ("ssel01", 16), ("sselp", 1),
               ("blk01s", 64)]:
    CF[_n] = (_o, _w)
    _o += _w
NCF = _o
CB = {}
_o = 0
for _n, _w in [("ones", 128), ("swam", 256), ("sel24", 8), ("pastm", 32), ("newm", 512)]:
    CB[_n] = (_o, _w)
    _o += _w
NCB = _o


def make_consts():
    cf = np.zeros((128, NCF), np.float32)
    cb = np.zeros((128, NCB), np.float32)

    def put(dst, tab, name, arr):
        o, w = tab[name]
        dst[:arr.shape[0], o:o + w] = arr

    i = np.arange(128)
    put(cf, CF, "ident", np.eye(128, dtype=np.float32))
    put(cf, CF, "ones", np.ones((128, 128), np.float32))
    le = (i[:, None] <= i[None, :]).astype(np.float32)
    gt = (i[:, None] > i[None, :]).astype(np.float32)
    put(cf, CF, "trip", le * (-1.0 / 16.0))
    put(cf, CF, "dp", gt * (-1.0 / 16.0))
    put(cf, CF, "mtp", le)
    put(cf, CF, "sselp", np.full((128, 1), -1.0 / 16.0, np.float32))
    j = np.arange(64)
    same = (j[:, None] // 4 == j[None, :] // 4)
    les = (same & (j[:, None] <= j[None, :])).astype(np.float32)
    gts = (same & (j[:, None] > j[None, :])).astype(np.float32)
    put(cf, CF, "tris", les * (-1.0 / 16.0))
    put(cf, CF, "ds", gts * (-1.0 / 16.0))
    put(cf, CF, "mts", les)
    sel = (j[:, None] // 4 == np.arange(16)[None, :]).astype(np.float32)
    put(cf, CF, "ssels", sel * (-1.0 / 16.0))
    put(cf, CF, "ssel01", sel)
    put(cf, CF, "blk01s", same.astype(np.float32))
    put(cb, CB, "ones", np.ones((128, 128), np.float32))
    put(cb, CB, "swam", np.concatenate([gt, le], axis=1))
    s24 = np.zeros((24, 8), np.float32)
    for h in range(8):
        s24[3 * h:3 * h + 3, h] = 1.0
    put(cb, CB, "sel24", s24)
    pm = (i[:, None] > (np.arange(32)[None, :] % 4)).astype(np.float32)
    put(cb, CB, "pastm", pm)
    nm = np.zeros((64, 16, 32), np.float32)
    for s_ in range(64):
        for c in range(32):
            if (s_ % 4) <= (c % 4):
                nm[s_, s_ // 4, c] = 1.0
    put(cb, CB, "newm", nm.reshape(64, 512))
    return cf, cb


NPP = 16 + 16 + 96 + 4 + 16
PP_G1, PP_G2, PP_BADA, PP_GNG, PP_SINK = 0, 16, 32, 128, 132


class Prog:
    def __init__(self, dbg=()):
        self.dbg = set(dbg)
        self.dbg_out = {}
        nc = bass.Bass("TRN2", target_bir_lowering=False)
        self.nc = nc
        k = K(nc)
        self.k = k
        dr = k.dram
        self.xT_in = dr("xT", [D, TTOK], F32, "ExternalInput")
        self.cT_in = dr("cT", [D, 1 + TS], F32, "ExternalInput")
        self.state_in = dr("state", [DEPTH, NSEQ_S, 4, 256, 512], F32, "ExternalInput")
        self.ckT_in = dr("ckT", [DEPTH, 64, 4, 2048 + 64], F32, "ExternalInput")
        self.ck_in = dr("ck", [DEPTH, NSEQ_S, 128, 256], F32, "ExternalInput")
        self.cv_in = dr("cv", [DEPTH, NSEQ_S, 128, 256], F32, "ExternalInput")
        self.w_ada = dr("w_ada", [DEPTH, D, 6 * D], F32, "ExternalInput")
        self.w_in = dr("w_in", [DEPTH, D, INW], F32, "ExternalInput")
        self.w_out = dr("w_out", [DEPTH, D, D], F32, "ExternalInput")
        self.peer_wq = dr("peer_wq", [DEPTH, D, D], F32, "ExternalInput")
        self.uT = dr("uT", [DEPTH, D, 16384], F32, "ExternalInput")
        self.pv = dr("pv", [DEPTH, 16384, D], F32, "ExternalInput")
        self.pkT_in = dr("pkT", [DEPTH, 128, 16, 128], F32, "ExternalInput")
        self.pp_in = dr("pp", [DEPTH, 128, NPP], F32, "ExternalInput")
        self.fg_in = dr("fg", [128, 16], F32, "ExternalInput")
        self.wal_in = dr("wal1", [DEPTH, 17, 1024], F32, "ExternalInput")
        self.cf_in = dr("cf", [128, NCF], F32, "ExternalInput")
        self.cb_in = dr("cb", [128, NCB], F32, "ExternalInput")
        self.yT_out = dr("yT", [D, TTOK], F32, "ExternalOutput")
        self.glap_out = dr("gla_p", [DEPTH, 4, 256, 512], F32, "ExternalOutput")
        self.kp_out = dr("k_p", [DEPTH, 128, 256], F32, "ExternalOutput")
        self.vp_out = dr("v_p", [DEPTH, 128, 256], F32, "ExternalOutput")
        self.glas_out = dr("gla_s", [DEPTH, NSEQ_S, 4, 256, 512], F32, "ExternalOutput")
        self.ks_out = dr("k_s", [DEPTH, NSEQ_S, 128, 256], F32, "ExternalOutput")
        self.vs_out = dr("v_s", [DEPTH, NSEQ_S, 128, 256], F32, "ExternalOutput")
        self.mods = dr("mods", [DEPTH, 128, 96, TS], F32, "Internal")

        self.banks = [k.ps(f"bank{i}") for i in range(8)]
        self.bank_i = 0
        self.slot_i = 0
        self.alloc()
        self.build()
        k.emit()

    def bank(self):
        b = self.banks[self.bank_i % 8]
        self.bank_i += 1
        return b

    def mm(self, out, lhsT, rhs, start=True, stop=True):
        self.k.op("tensor", lambda e: e.matmul(out.ap, lhsT=lhsT.ap, rhs=rhs.ap, start=start, stop=stop),
                  [lhsT.tile, rhs.tile], [out.tile])

    def transpose(self, out, in_, ident):
        self.k.op("tensor", lambda e: e.transpose(out.ap, in_.ap, ident.ap), [in_.tile, ident.tile], [out.tile])

    def act(self, out, in_, func, scale=None, bias=None, alpha=None, accum=None):
        kw = {}
        reads = [in_.tile]
        writes = [out.tile]
        if scale is not None:
            if isinstance(scale, V):
                kw["scale"] = scale.ap
                reads.append(scale.tile)
            else:
                kw["scale"] = float(scale)
        if bias is not None:
            if isinstance(bias, V):
                kw["bias"] = bias.ap
                reads.append(bias.tile)
            else:
                kw["bias"] = float(bias)
        if alpha is not None:
            kw["alpha"] = float(alpha)
        if accum is not None:
            kw["accum_out"] = accum.ap
            writes.append(accum.tile)
        self.k.op("scalar", lambda e: e.activation(out=out.ap, in_=in_.ap, func=func, **kw), reads, writes)

    def tt(self, out, a, b, op, eng="vector"):
        self.k.op(eng, lambda e: e.tensor_tensor(out=out.ap, in0=a.ap, in1=b.ap, op=op),
                  [a.tile, b.tile], [out.tile])

    def ts(self, out, a, s1, op0, s2=None, op1=None, eng="vector"):
        reads = [a.tile]
        a1 = s1
        a2 = s2
        if isinstance(s1, V):
            reads.append(s1.tile)
            a1 = s1.ap
        if isinstance(s2, V):
            reads.append(s2.tile)
            a2 = s2.ap
        if op1 is None:
            self.k.op(eng, lambda e: e.tensor_scalar(out=out.ap, in0=a.ap, scalar1=a1, scalar2=None, op0=op0),
                      reads, [out.tile])
        else:
            self.k.op(eng, lambda e: e.tensor_scalar(out=out.ap, in0=a.ap, scalar1=a1, scalar2=a2, op0=op0, op1=op1),
                      reads, [out.tile])

    def stt(self, out, a, s, b, op0, op1):
        reads = [a.tile, b.tile]
        sa = s
        if isinstance(s, V):
            reads.append(s.tile)
            sa = s.ap
        self.k.op("vector", lambda e: e.scalar_tensor_tensor(out=out.ap, in0=a.ap, scalar=sa, in1=b.ap, op0=op0, op1=op1),
                  reads, [out.tile])

    def cp(self, out, in_, eng="vector"):
        if eng == "scalar":
            self.act(out, in_, AF.Copy)
        else:
            self.k.op(eng, lambda e: e.tensor_copy(out=out.ap, in_=in_.ap), [in_.tile], [out.tile])

    def memset(self, out, val, eng="vector"):
        self.k.op(eng, lambda e: e.memset(out.ap, val), [], [out.tile])

    def recip(self, out, in_):
        self.k.op("vector", lambda e: e.reciprocal(out=out.ap, in_=in_.ap), [in_.tile], [out.tile])

    def max8(self, out, in_):
        self.k.op("vector", lambda e: e.max(out=out.ap, in_=in_.ap), [in_.tile], [out.tile])

    def mrep(self, out, rep, vals):
        self.k.op("vector", lambda e: e.match_replace(out=out.ap, in_to_replace=rep.ap, in_values=vals.ap, imm_value=NEG),
                  [rep.tile, vals.tile], [out.tile])

    def dma(self, out, in_, eng="sync"):
        self.k.dma(eng, out, in_)

    def dump(self, name, v, dt=F32):
        if name not in self.dbg:
            return
        shp = list(v.shape)
        o = self.k.dram("dbg_" + name, shp, dt, "ExternalOutput")
        self.dbg_out[name] = o
        self.dma(o.v, v, eng="gpsimd" if dt != v.ap.dtype else "sync")

    def alloc(self):
        k = self.k
        AB = 206 * 1024
        k.make_arena(AB)
        o = 0

        def take(nb):
            nonlocal o
            r = o
            o += (nb + 31) // 32 * 32
            return r

        c = k.carve
        self.cf = c(take(NCF * 4), [128, NCF], F32, "cf")
        self.cb = c(take(NCB * 2), [128, NCB], BF16, "cb")
        self.S = [c(take(16384), [128, 8, 512], F32, f"S{l}") for l in range(DEPTH)]
        self.pskT = [c(take(1024), [128, 4, 128], BF16, f"pskT{l}") for l in range(DEPTH)]
        self.psv = [c(take(512), [128, 256], BF16, f"psv{l}") for l in range(DEPTH)]
        self.pp = [c(take(NPP * 4), [128, NPP], F32, f"pp{l}") for l in range(DEPTH)]
        self.fg = c(take(64), [128, 16], F32, "fg")
        self.modp = [c(take(96 * 4), [128, 96], F32, f"modp{l}") for l in range(DEPTH)]
        self.A1 = [c(take(64), [128, 16], F32, f"A1{l}") for l in range(DEPTH)]
        self.A2 = [c(take(64), [128, 16], F32, f"A2{l}") for l in range(DEPTH)]
        self.esink = [c(take(64), [128, 16], F32, f"esink{l}") for l in range(DEPTH)]
        self.smallf = c(take(1024), [128, 256], F32, "smallf")
        self.slots = [c(take(16384), [128, 16, 512], BF16, f"slot{i}") for i in range(2)]
        self.base = o
        self.layouts = {512: self.layout(512), 64: self.layout(64)}
        self.newm = self.cbv("newm", 64).re("p (b c) -> p b c", b=16)

    def layout(self, TT):
        k = self.k
        c = k.carve
        o = self.base
        NST = 1 if TT == 64 else 4
        TP = min(TT, 128)
        B = {}

        def take(nb):
            nonlocal o
            r = o
            o += (nb + 31) // 32 * 32
            return r

        B["xT"] = c(take(16 * TT * 4), [128, 16, TT], F32, "xT")
        B["hT"] = c(take(16 * TT * 2), [128, 16, TT], BF16, "hT")
        ph = o
        B["sq"] = [c(take(TT * 4), [128, TT], F32, "sq") for _ in range(2)]
        B["rstd"] = c(take(TT * 4), [128, TT], F32, "rstd")
        B["ntmp"] = [c(take(TT * 4), [128, TT], F32, "ntmp") for _ in range(2)]
        o = ph
        B["qT"] = c(take(8 * TT * 2), [128, 8, TT], BF16, "qT")
        B["kT"] = c(take(8 * TT * 2), [128, 8, TT], BF16, "kT")
        B["ktok"] = c(take(NST * 1024 * 2), [TP, NST, 1024], BF16, "ktok")
        vt_off = take(NST * 2048 * 2)
        B["vtok"] = c(vt_off, [TP, NST, 2048], BF16, "vtok")
        B["onT"] = c(take(16 * TT * 2), [128, 16, TT], BF16, "onT")
        B["glrT1"] = c(take(TT * 2), [17, TT], BF16, "glrT1")
        B["wal1"] = c(take(1024 * 2), [17, 1024], BF16, "wal1")
        tr = o
        B["l"] = c(take(4096), [TP, 1024], F32, "l")
        B["ebe"] = B["l"]
        B["eb"] = c(take(8 * TP * 4), [128, 8, TP], F32, "eb")
        B["enb"] = c(take(8 * TP * 4), [128, 8, TP], F32, "enb")
        B["khat"] = c(take(2048), [TP, 1024], BF16, "khat")
        B["attT"] = c(take(4 * TP * 2), [TP, 4, TP], BF16, "attT")
        B["osq"] = c(take(4 * TP * 4), [128, 4, TP], F32, "osq")
        B["orstd"] = c(take(TP * 4), [128, TP], F32, "orstd")
        B["ebend"] = c(take(8 * 16 * 4), [128, 8, 16], F32, "ebend")
        if TT == 64:
            B["S0"] = [c(take(4096), [128, 2, 512], F32, "S0") for _ in range(2)]
            B["Snew"] = [c(take(2048), [128, 512], F32, "Snew") for _ in range(2)]
            B["khm"] = [c(take(2048), [TP, 1024], BF16, "khm") for _ in range(2)]
            B["ckT"] = c(take(64 * 128 * 2), [128, 64, 128], BF16, "ckT")
            B["cv"] = c(take(16 * 256 * 2), [128, 16, 256], BF16, "cv")
            B["knew"] = c(take(512 * 4), [TP, 512], F32, "knew")
            B["mA"] = c(take(16 * TT * 4), [128, 16, TT], F32, "mA")
            B["mS"] = c(take(16 * TT * 4), [128, 16, TT], F32, "mS")
            B["mG"] = c(take(16 * TT * 4), [128, 16, TT], F32, "mG")
            B["PTs"] = [c(take(64), [128, 32], BF16, "PTs") for _ in range(2)]
            B["PTn"] = [c(take(64), [TP, 32], BF16, "PTn") for _ in range(2)]
            B["rtmp"] = [c(take(TT * 4), [128, TT], F32, "rtmp") for _ in range(2)]
            tr = o
        end_mixer = o
        o = ph
        B["sqT"] = c(take(16 * TT * 2), [128, 16, TT], BF16, "sqT")
        o = vt_off
        B["oswaT"] = c(take(16 * TT * 2), [128, 16, TT], BF16, "oswaT")
        o = tr
        B["skT"] = c(take(4 * (128 + TT) * 2), [128, 4, 128 + TT], BF16, "skT")
        B["sv1"] = c(take((NST + 1) * 256 * 2), [TP, NST + 1, 256], BF16, "sv1")
        B["PT"] = [c(take(512), [128, 256], BF16, "PT") for _ in range(2)]
        B["kvo"] = c(take(512 * 4), [TP, 512], F32, "kvo")
        B["rden"] = [c(take(512), [128, 128], F32, "rden") for _ in range(2)]
        B["gtmp"] = [c(take(TT * 4), [128, TT], F32, "gtmp") for _ in range(2)]
        end_mixer = max(end_mixer, o)
        o = ph
        B["pqT"] = c(take(16 * TT * 2), [128, 16, TT], BF16, "pqT")
        B["gbcb"] = c(take(8 * TT * 2), [128, 8, TT], BF16, "gbcb")
        B["pkT"] = c(take(16 * 128 * 2), [128, 16, 128], BF16, "pkT")
        B["thT"] = c(take(TT * 2), [128, TT], BF16, "thT")
        B["gfT"] = c(take(TT * 4), [8, TT], F32, "gfT")
        pk = o
        B["sc"] = c(take(16 * 128 * 4), [TP, 16, 128], F32, "sc")
        B["scr"] = c(take(512), [TP, 128], F32, "scr")
        B["top16"] = c(take(16 * 16 * 4), [TP, 16, 16], F32, "top16")
        B["comb"] = [c(take(1024), [TP, 256], F32, "comb") for _ in range(3)]
        B["c24"] = c(take(8 * 24 * 4), [TP, 8, 24], F32, "c24")
        B["pm"] = c(take(256 * 4), [TP, 256], F32, "pm")
        B["pmb"] = c(take(64 * 2), [TP, 64], BF16, "pmb")
        o = pk
        B["hraw"] = [c(take(4 * TT * 4), [128, 4, TT], F32, "hraw") for _ in range(2)]
        B["zc"] = [c(take(TT * 4), [128, TT], F32, "zc") for _ in range(2)]
        B["E"] = [c(take(TT * 2), [128, TT], BF16, "E") for _ in range(4)]
        B["ptmp"] = [c(take(TT * 2), [128, TT], BF16, "ptmp") for _ in range(4)]
        B["acc"] = [c(take(TT * 2), [128, TT], BF16, "acc") for _ in range(2)]
        B["AT"] = [c(take(4 * TT * 2), [128, 4, TT], BF16, "AT") for _ in range(2)]
        end_peer = o
        B["_end"] = max(end_mixer, end_peer)
        return B

    def slot(self):
        s = self.slots[self.slot_i % 2]
        self.slot_i += 1
        return s

    def wload(self, s, W, l, c0, n, dst=0):
        src = V(W, W.t[l].rearrange("(kc p) n -> p kc n", p=128)[:, :, c0:c0 + n])
        self.dma(s[:, :, dst:dst + n], src, eng="gpsimd")

    def proj_fm(self, s, ncols, hT, TT, consume, col0=0):
        nch = (ncols + 127) // 128
        for cc in range(nch):
            M = min(128, ncols - cc * 128)
            ps = self.bank()
            for kc in range(16):
                self.mm(ps[0:M, 0:TT], s[:, kc, col0 + cc * 128:col0 + cc * 128 + M], hT[:, kc, 0:TT],
                        start=(kc == 0), stop=(kc == 15))
            consume(cc, ps[0:M, 0:TT])

    def proj_tm(self, s, ncols, hT, subt, consume):
        for si, (t0, n) in enumerate(subt):
            ps = self.bank()
            for kc in range(16):
                self.mm(ps[0:n, 0:ncols], hT[:, kc, t0:t0 + n], s[:, kc, 0:ncols], start=(kc == 0), stop=(kc == 15))
            consume(si, ps[0:n, 0:ncols])

    def cfv(self, name, rows=128):
        o, w = CF[name]
        return self.cf[0:rows, o:o + w]

    def cbv(self, name, rows=128):
        o, w = CB[name]
        return self.cb[0:rows, o:o + w]

    def build(self):
        self.dma(self.cf.v, self.cf_in.v)
        self.dma(self.cb.v, self.cb_in.v, eng="gpsimd")
        self.dma(self.fg.v, self.fg_in.v)
        for l in range(DEPTH):
            self.dma(self.pp[l].v, self.pp_in[l])
            self.act(self.esink[l].v, self.pp[l][:, PP_SINK:PP_SINK + 16], AF.Exp)
        self.memset(self.smallf[:, 0:1], EPS)
        self.memset(self.smallf[:, 1:2], 1.0)
        self.compute_mod()
        for l in range(DEPTH):
            self.dump(f"modp{l}", self.modp[l].v)
        tiles = [("s", SEQ, TS)] + [("p", i * 512, 512) for i in range(4)]
        if "tiles_s" in FLAGS:
            tiles = tiles[:1]
        if "tiles_p0" in FLAGS:
            tiles = tiles[1:2]
        if "tiles_sp0" in FLAGS:
            tiles = tiles[0:2]
        for (kind, t0, TT) in tiles:
            self.run_tile(kind, t0, TT)

    def compute_mod(self):
        B = self.layouts[512]
        NCOL = 1 + TS
        cs = B["xT"]
        sc = B["hT"]
        stage = B["onT"]
        stg = B["hraw"][0].v.re("p a b -> p (a b)").re("p (a b) -> p a b", a=8)
        self.dma(cs[:, :, 0:NCOL], V(self.cT_in, self.cT_in.t.rearrange("(kc p) n -> p kc n", p=128)))
        self.act(sc[:, :, 0:NCOL], cs[:, :, 0:NCOL], AF.Silu)
        for l in range(DEPTH):
            for g in range(24):
                s = self.slot()
                self.wload(s, self.w_ada, l, g * 512, 512)

                def consume(cc, ps, g=g, l=l):
                    ch = g * 4 + cc
                    self.act(self.modp[l][:, ch:ch + 1], ps[:, 0:1], AF.Identity,
                             bias=self.pp[l][:, PP_BADA + ch:PP_BADA + ch + 1])
                    self.act(stg[:, ch % 8, 0:TS], ps[:, 1:NCOL], AF.Identity,
                             bias=self.pp[l][:, PP_BADA + ch:PP_BADA + ch + 1])
                    if ch % 8 == 7:
                        c8 = ch - 7
                        self.dma(self.mods[l][:, c8:c8 + 8, :], stg[:, :, 0:TS])
                self.proj_fm(s, 512, sc, NCOL, consume)
            self.stt(self.A1[l].v, self.modp[l][:, 16:32], 1.0, self.pp[l][:, PP_G1:PP_G1 + 16], ALU.add, ALU.mult)
            self.stt(self.A2[l].v, self.modp[l][:, 64:80], 1.0, self.pp[l][:, PP_G2:PP_G2 + 16], ALU.add, ALU.mult)

    def run_tile(self, kind, t0, TT):
        B = self.layouts[TT]
        xsrc = V(self.xT_in, self.xT_in.t.rearrange("(c p) t -> p c t", p=128)[:, :, t0:t0 + TT])
        self.dma(B["xT"].v, xsrc)
        for l in range(1 if "depth1" in FLAGS else DEPTH):
            self.layer(kind, t0, TT, l, B)
        self.final_norm(kind, t0, TT, B)

    def rms_rstd(self, B, TT):
        xT = B["xT"]
        ps = self.bank()
        for c in range(16):
            sq = B["sq"][c % 2]
            self.act(sq.v, xT[:, c, :], AF.Square)
            self.mm(ps[:, 0:TT], self.cfv("ones"), sq.v, start=(c == 0), stop=(c == 15))
        self.act(B["rstd"].v, ps[:, 0:TT], AF.Ln, scale=1.0 / D, bias=self.smallf[:, 0:1])
        self.act(B["rstd"].v, B["rstd"].v, AF.Exp, scale=-0.5)

    def load_mod_s(self, B, l, which):
        return V(self.mods, self.mods.t[l][:, which * 16:(which + 1) * 16, :])

    def norm_mod(self, kind, B, TT, l, which):
        self.rms_rstd(B, TT)
        xT, hT = B["xT"], B["hT"]
        if kind == "p":
            A = (self.A1 if which == 1 else self.A2)[l]
            sh0 = 0 if which == 1 else 48
            for c in range(16):
                tmp = B["ntmp"][c % 2]
                self.stt(tmp.v, xT[:, c, :], A[:, c:c + 1], B["rstd"].v, ALU.mult, ALU.mult)
                self.act(hT[:, c, :], tmp.v, AF.Identity, bias=self.modp[l][:, sh0 + c:sh0 + c + 1])
        else:
            g0 = PP_G1 if which == 1 else PP_G2
            sc_i, sh_i = (1, 0) if which == 1 else (4, 3)
            self.dma(B["mA"].v, self.load_mod_s(B, l, sc_i))
            self.dma(B["mS"].v, self.load_mod_s(B, l, sh_i))
            for c in range(16):
                self.ts(B["mA"][:, c, :], B["mA"][:, c, :], 1.0, ALU.add,
                        self.pp[l][:, g0 + c:g0 + c + 1], ALU.mult)
                tmp = B["ntmp"][c % 2]
                self.tt(tmp.v, xT[:, c, :], B["rstd"].v, ALU.mult)
                self.tt(tmp.v, tmp.v, B["mA"][:, c, :], ALU.mult)
                self.tt(hT[:, c, :], tmp.v, B["mS"][:, c, :], ALU.add)

    def residual(self, kind, B, l, which, c, ps):
        xT = B["xT"]
        if kind == "p":
            g0 = 32 if which == 1 else 80
            self.stt(xT[:, c, :], ps, self.modp[l][:, g0 + c:g0 + c + 1], xT[:, c, :], ALU.mult, ALU.add)
        else:
            tmp = B["rtmp"][c % 2]
            self.tt(tmp.v, ps, B["mG"][:, c, :], ALU.mult)
            self.tt(xT[:, c, :], xT[:, c, :], tmp.v, ALU.add)

    def final_norm(self, kind, t0, TT, B):
        self.rms_rstd(B, TT)
        ydst = self.yT_out.t.rearrange("(c p) t -> p c t", p=128)
        for c in range(16):
            tmp = B["ntmp"][c % 2]
            self.stt(tmp.v, B["xT"][:, c, :], self.fg[:, c:c + 1], B["rstd"].v, ALU.mult, ALU.mult)
            self.dma(V(self.yT_out, ydst[:, c, t0:t0 + TT]), tmp.v)

    def bank_ex(self, exclude):
        while True:
            b = self.banks[self.bank_i % 8]
            self.bank_i += 1
            if (self.bank_i - 1) % 8 not in exclude:
                return b

    def layer(self, kind, t0, TT, l, B):
        if "nomixer" not in FLAGS:
            self.mixer(kind, t0, TT, l, B)
        self.dump(f"x1{kind}{l}_{t0}", B["xT"].v)
        if "nopeer" not in FLAGS:
            self.peer(kind, t0, TT, l, B)
        self.dump(f"x2{kind}{l}_{t0}", B["xT"].v)

    def mixer(self, kind, t0, TT, l, B):
        P = kind == "p"
        subt = [(i * 128, 128) for i in range(4)] if P else [(0, TS)]
        hT = B["hT"]
        self.norm_mod(kind, B, TT, l, 1)
        self.dump(f"h{kind}{l}_{t0}", hT.v, BF16)
        self.dma(B["wal1"].v, self.wal_in[l], eng="gpsimd")
        self.memset(B["glrT1"].v, 1.0)
        s = self.slot()
        self.wload(s, self.w_in, l, OFF_GLR, 16)
        self.proj_fm(s, 16, hT, TT, lambda cc, ps: self.cp(B["glrT1"][0:16, :], ps, eng="scalar"))
        for g in range(2):
            s = self.slot()
            self.wload(s, self.w_in, l, OFF_GQ + g * 512, 512)
            self.proj_fm(s, 512, hT, TT, lambda cc, ps, g=g: self.cp(B["qT"][:, g * 4 + cc, :], ps, eng="scalar"))
        for g in range(2):
            s = self.slot()
            self.wload(s, self.w_in, l, OFF_GK + g * 512, 512)
            self.proj_fm(s, 512, hT, TT, lambda cc, ps, g=g: self.cp(B["kT"][:, g * 4 + cc, :], ps, eng="vector"))
            self.proj_tm(s, 512, hT, subt,
                         lambda si, ps, g=g: self.cp(B["ktok"][:, si, g * 512:(g + 1) * 512], ps, eng="scalar"))
        for g in range(4):
            s = self.slot()
            self.wload(s, self.w_in, l, OFF_GV + g * 512, 512)
            self.proj_tm(s, 512, hT, subt,
                         lambda si, ps, g=g: self.cp(B["vtok"][:, si, g * 512:(g + 1) * 512], ps,
                                                     eng="vector" if si % 2 else "scalar"))
        for ci, (c0, C) in enumerate(subt):
            if "nogla" not in FLAGS:
                self.gla_chunk(kind, l, B, ci, c0, C, t0)
        if P and t0 + TT == SEQ:
            dst = self.glap_out.t[l].rearrange("h (dc p) e -> p (h dc) e", p=128)
            self.dma(V(self.glap_out, dst), self.S[l].v)
        if "noswa" in FLAGS:
            pass
        elif P:
            self.swa_prompt(l, B, t0, TT)
        else:
            self.swa_sample(l, B)
        for (off, fn, mode) in ((OFF_GR, AF.Silu, 0), (OFF_GA, AF.Sigmoid, 0), (OFF_GB, AF.Sigmoid, 1)):
            for g in range(4):
                s = self.slot()
                self.wload(s, self.w_in, l, off + g * 512, 512)

                def c_gate(cc, ps, g=g, fn=fn, mode=mode):
                    ch = g * 4 + cc
                    tmp = B["gtmp"][cc % 2]
                    self.act(tmp.v, ps, fn)
                    if mode == 0:
                        self.tt(B["onT"][:, ch, :], B["onT"][:, ch, :], tmp.v, ALU.mult)
                    else:
                        self.tt(tmp.v, tmp.v, B["oswaT"][:, ch, :], ALU.mult)
                        self.tt(B["onT"][:, ch, :], B["onT"][:, ch, :], tmp.v, ALU.add)
                self.proj_fm(s, 512, hT, TT, c_gate)
        if not P:
            self.dma(B["mG"].v, self.load_mod_s(B, l, 2))
        for g in range(4):
            s = self.slot()
            self.wload(s, self.w_out, l, g * 512, 512)
            self.proj_fm(s, 512, B["onT"], TT, lambda cc, ps, g=g: self.residual(kind, B, l, 1, g * 4 + cc, ps))

    def gla_chunk(self, kind, l, B, ci, c0, C, t0):
        P = kind == "p"
        nseq = 1 if P else NSEQ_S
        tri = self.cfv("trip") if P else self.cfv("tris", 64)
        dmat = self.cfv("dp") if P else self.cfv("ds", 64)
        mt = self.cfv("mtp") if P else self.cfv("mts", 64)
        ssel = self.cfv("sselp") if P else self.cfv("ssels", 64)
        first = P and t0 == 0 and ci == 0
        L_ = B["l"]
        eb, enb = B["eb"], B["enb"]
        for hf in range(2):
            ps = self.bank()
            self.mm(ps[0:C, :], B["glrT1"][:, c0:c0 + C], B["wal1"][:, hf * 512:(hf + 1) * 512])
            self.act(L_[:, hf * 512:(hf + 1) * 512], ps[0:C, :], AF.Exp, scale=-1.0)
        self.act(L_.v, L_.v, AF.Ln, bias=self.smallf[0:C, 1:2])
        psb = [self.bank(), self.bank()]
        for dc in range(8):
            self.mm(psb[dc // 4][:, (dc % 4) * C:(dc % 4 + 1) * C], L_[:, dc * 128:(dc + 1) * 128], tri)
        for hf in range(2):
            pv_ = psb[hf][:, 0:4 * C].re("p (a b) -> p a b", a=4)
            self.act(eb[:, hf * 4:(hf + 1) * 4, :], pv_, AF.Exp)
            self.act(enb[:, hf * 4:(hf + 1) * 4, :], pv_, AF.Exp, scale=-1.0)
        self.stt(eb.v, eb.v, 0.0625, B["qT"][:, :, c0:c0 + C], ALU.mult, ALU.mult)
        self.tt(enb.v, enb.v, B["kT"][:, :, c0:c0 + C], ALU.mult)
        pse = self.bank()
        for dc in range(8):
            self.mm(pse[:, dc * nseq:(dc + 1) * nseq], L_[:, dc * 128:(dc + 1) * 128], ssel)
        self.act(B["ebend"][:, :, 0:nseq], pse[:, 0:8 * nseq].re("p (a b) -> p a b", a=8), AF.Exp)
        pq = [self.bank(), self.bank()]
        for hf in range(2):
            self.mm(pq[hf][0:C, :], dmat, L_[:, hf * 512:(hf + 1) * 512])
        for hf in range(2):
            self.act(B["ebe"][:, hf * 512:(hf + 1) * 512], pq[hf][0:C, :], AF.Exp)
        self.tt(B["khat"].v, B["ktok"][:, ci, :], B["ebe"].v, ALU.mult)
        psa = self.bank()
        for h in range(4):
            for dc in range(2):
                self.mm(psa[0:C, h * C:(h + 1) * C], enb[:, 2 * h + dc, :], eb[:, 2 * h + dc, :],
                        start=(dc == 0), stop=(dc == 1))
        self.tt(B["attT"].v, psa[0:C, 0:4 * C].re("p (h c) -> p h c", h=4), mt.unsq(1).bc([C, 4, C]), ALU.mult)
        for h in range(4):
            if P:
                pso = self.bank()
                oviews = [pso[:, ec * C:(ec + 1) * C] for ec in range(4)]
                for ec in range(4):
                    self.mm(oviews[ec], B["vtok"][:, ci, h * 512 + ec * 128:h * 512 + (ec + 1) * 128],
                            B["attT"][:, h, :], start=True, stop=first)
                    if not first:
                        for dc in range(2):
                            self.mm(oviews[ec], self.S[l][:, 2 * h + dc, ec * 128:(ec + 1) * 128],
                                    eb[:, 2 * h + dc, :], start=False, stop=(dc == 1))
                excl = set()
            else:
                oviews = [self.banks[ec][:, 0:C] for ec in range(4)]
                excl = {0, 1, 2, 3}
                for ec in range(4):
                    self.mm(oviews[ec], B["vtok"][:, 0, h * 512 + ec * 128:h * 512 + (ec + 1) * 128],
                            B["attT"][:, h, :], start=True, stop=False)
                for b in range(NSEQ_S):
                    S0 = B["S0"][b % 2]
                    self.dma(S0.v, V(self.state_in, self.state_in.t[l, b, h].rearrange("(dc p) e -> p dc e", p=128)))
                    for ec in range(4):
                        for dc in range(2):
                            self.mm(self.banks[ec][:, LS * b:LS * b + LS], S0[:, dc, ec * 128:(ec + 1) * 128],
                                    eb[:, 2 * h + dc, LS * b:LS * b + LS], start=False,
                                    stop=(b == NSEQ_S - 1 and dc == 1))
                    khm = B["khm"][b % 2]
                    self.ts(khm[:, 0:256], B["khat"][:, h * 256:(h + 1) * 256],
                            self.cfv("ssel01", 64)[:, b:b + 1], ALU.mult)
                    for dc in range(2):
                        pss = self.bank_ex(excl)
                        self.mm(pss[:, :], khm[:, dc * 128:(dc + 1) * 128], B["vtok"][:, 0, h * 512:(h + 1) * 512])
                        sn = B["Snew"][dc]
                        self.stt(sn.v, S0[:, dc, :], B["ebend"][:, 2 * h + dc, b:b + 1], pss[:, :], ALU.mult, ALU.add)
                        self.dma(V(self.glas_out, self.glas_out.t[l, b, h, dc * 128:(dc + 1) * 128, :]), sn.v)
            osq = B["osq"]
            for ec in range(4):
                self.act(osq[:, ec, 0:C], oviews[ec], AF.Square)
            psn = self.bank_ex(excl)
            for ec in range(4):
                self.mm(psn[:, 0:C], self.cfv("ones"), osq[:, ec, 0:C], start=(ec == 0), stop=(ec == 3))
            ors = B["orstd"][:, 0:C]
            self.act(ors, psn[:, 0:C], AF.Ln, scale=1.0 / 512.0, bias=self.smallf[:, 0:1])
            self.act(ors, ors, AF.Exp, scale=-0.5)
            for ec in range(4):
                self.stt(B["onT"][:, 4 * h + ec, c0:c0 + C], oviews[ec],
                         self.pp[l][:, PP_GNG + ec:PP_GNG + ec + 1], ors, ALU.mult, ALU.mult)
            if P:
                for dc in range(2):
                    idx = 2 * h + dc
                    pss = self.bank()
                    self.mm(pss[:, :], B["khat"][:, idx * 128:(idx + 1) * 128], B["vtok"][:, ci, h * 512:(h + 1) * 512])
                    if first:
                        self.cp(self.S[l][:, idx, :], pss[:, :], eng="scalar")
                    else:
                        self.stt(self.S[l][:, idx, :], self.S[l][:, idx, :], B["ebend"][:, idx, 0:1], pss[:, :],
                                 ALU.mult, ALU.add)

    def swa_proj(self, l, B, TT, subt, c_kv):
        hT = B["hT"]
        if "sp_nosq" not in FLAGS:
            for g in range(4):
                s = self.slot()
                self.wload(s, self.w_in, l, OFF_SQ + g * 512, 512)
                self.proj_fm(s, 512, hT, TT, lambda cc, ps, g=g: self.cp(B["sqT"][:, g * 4 + cc, :], ps,
                                                                         eng="scalar" if cc % 2 else "vector"))
        s = self.slot()
        self.wload(s, self.w_in, l, OFF_SK, 512)
        if "sp_notm" not in FLAGS:
            self.proj_tm(s, 512, hT, subt, c_kv)
        if "sp_nodup" in FLAGS:
            return
        s2 = self.slot()
        for kv in range(4):
            for dup in range(2):
                self.cp(s2[:, :, kv * 128 + dup * 64:kv * 128 + dup * 64 + 64], s[:, :, kv * 64:(kv + 1) * 64],
                        eng="vector")
        self.proj_fm(s2, 512, hT, TT, lambda cc, ps: self.cp(B["skT"][:, cc, 128:128 + TT], ps, eng="scalar"))

    def swa_prompt(self, l, B, t0, TT):
        first_tile = t0 == 0
        last_tile = t0 + TT == SEQ
        subt = [(i * 128, 128) for i in range(4)]
        if not first_tile:
            self.cp(B["skT"][:, :, 0:128], self.pskT[l].v, eng="gpsimd")
            self.cp(B["sv1"][:, 0, :], self.psv[l].v, eng="gpsimd")

        def c_kv(si, ps):
            self.cp(B["sv1"][:, si + 1, :], ps[:, 256:512], eng="vector")
            if last_tile and si == 3:
                self.cp(B["kvo"].v, ps, eng="scalar")
                self.dma(self.kp_out[l], B["kvo"][:, 0:256])
                self.dma(self.vp_out[l], B["kvo"][:, 256:512])
        self.swa_proj(l, B, TT, subt, c_kv)
        if not last_tile:
            self.cp(self.pskT[l].v, B["skT"][:, :, TT:TT + 128], eng="gpsimd")
            self.cp(self.psv[l].v, B["sv1"][:, 4, :], eng="gpsimd")
        numb, denb = self.banks[0], self.banks[1]
        excl = {0, 1}
        onesb = self.cbv("ones")[:, 0:64]
        swam = self.cbv("swam")
        it = 0
        for chunk in range(16):
            kv = chunk // 4
            for half in range(2):
                pr = slice(64 * half, 64 * half + 64)
                for qb in range(4):
                    has_prev = not (first_tile and qb == 0)
                    ps = self.bank_ex(excl)
                    q = B["sqT"][pr, chunk, qb * 128:(qb + 1) * 128]
                    if has_prev:
                        self.mm(ps[:, 0:128], B["skT"][pr, kv, qb * 128:(qb + 1) * 128], q)
                    self.mm(ps[:, 128:256], B["skT"][pr, kv, (qb + 1) * 128:(qb + 2) * 128], q)
                    PT = B["PT"][it % 2]
                    it += 1
                    lo = 0 if has_prev else 128
                    self.act(PT[:, lo:256], ps[:, lo:256], AF.Exp, scale=0.125)
                    self.tt(PT[:, lo:256], PT[:, lo:256], swam[:, lo:256], ALU.mult, eng="gpsimd")
                    n_ = numb[pr, qb * 128:(qb + 1) * 128]
                    d_ = denb[pr, qb * 128:(qb + 1) * 128]
                    v_prev = B["sv1"][:, qb, kv * 64:(kv + 1) * 64]
                    v_cur = B["sv1"][:, qb + 1, kv * 64:(kv + 1) * 64]
                    if has_prev:
                        self.mm(n_, v_prev, PT[:, 0:128], start=True, stop=False)
                        self.mm(n_, v_cur, PT[:, 128:256], start=False, stop=True)
                        self.mm(d_, onesb, PT[:, 0:128], start=True, stop=False)
                        self.mm(d_, onesb, PT[:, 128:256], start=False, stop=True)
                    else:
                        self.mm(n_, v_cur, PT[:, 128:256])
                        self.mm(d_, onesb, PT[:, 128:256])
            g0 = B["gtmp"][chunk % 2]
            self.ts(g0.v, denb[:, 0:TT], self.esink[l][:, chunk:chunk + 1], ALU.add)
            self.recip(g0.v, g0.v)
            self.tt(B["oswaT"][:, chunk, :], numb[:, 0:TT], g0.v, ALU.mult)

    def swa_sample(self, l, B):
        TT = TS
        subt = [(0, TS)]

        def c_kv(si, ps):
            self.cp(B["sv1"][:, 1, :], ps[:, 256:512], eng="vector")
            self.cp(B["knew"].v, ps, eng="scalar")
        if "swa_noproj" not in FLAGS:
            self.swa_proj(l, B, TT, subt, c_kv)
        cksrc = self.ckT_in[l][:, :, 0:2048]
        if "swa_nocache" not in FLAGS:
            self.dma(B["ckT"][0:64].re("p (a c) b -> p a (c b)", c=16), cksrc, eng="gpsimd")
            self.dma(B["ckT"][64:128].re("p (a c) b -> p a (c b)", c=16), cksrc, eng="gpsimd")
        if "swa_nocv" not in FLAGS:
            self.dma(B["cv"].v, V(self.cv_in, self.cv_in.t[l].rearrange("b k c -> k b c")), eng="gpsimd")
        if "swa_nodd" not in FLAGS:
            self.dma(self.ks_out[l][:, 0:124, :], self.ck_in[l][:, 4:128, :])
            self.dma(self.vs_out[l][:, 0:124, :], self.cv_in[l][:, 4:128, :])
        for b in range(NSEQ_S if "swa_noout" not in FLAGS else 0):
            self.dma(self.ks_out[l][b, 124:128, :], B["knew"][LS * b:LS * b + LS, 0:256])
            self.dma(self.vs_out[l][b, 124:128, :], B["knew"][LS * b:LS * b + LS, 256:512])
        onesb = self.cbv("ones")[:, 0:64]
        it = 0
        for b in range(NSEQ_S if "swa_nobody" not in FLAGS else 0):
            for kv in range(4):
                pss = self.bank()
                psn_ = self.bank()
                for j in range(8):
                    hq = 8 * kv + j
                    chunk, half = hq // 2, hq % 2
                    pr = slice(64 * half, 64 * half + 64)
                    q = B["sqT"][pr, chunk, LS * b:LS * b + LS]
                    self.mm(pss[:, 4 * j:4 * j + 4], B["ckT"][pr, b * 4 + kv, :], q)
                    self.mm(psn_[0:TS, 4 * j:4 * j + 4], B["skT"][pr, kv, 128:128 + TS], q)
                PTs = B["PTs"][it % 2]
                PTn = B["PTn"][it % 2]
                it += 1
                self.act(PTs.v, pss[:, 0:32], AF.Exp, scale=0.125)
                self.tt(PTs.v, PTs.v, self.cbv("pastm"), ALU.mult, eng="gpsimd")
                self.act(PTn.v, psn_[0:TS, 0:32], AF.Exp, scale=0.125)
                self.tt(PTn.v, PTn.v, self.newm[:, b, :], ALU.mult, eng="gpsimd")
                nb = self.bank()
                db = self.bank()
                for half in range(2):
                    pr = slice(64 * half, 64 * half + 64)
                    pc = PTs.v.re("p (j i) -> p j i", i=4)[:, half::2, :]
                    nc_ = PTn.v.re("p (j i) -> p j i", i=4)[:, half::2, :]
                    n_ = nb[pr, 0:16].re("p (a b) -> p a b", b=4)
                    d_ = db[pr, 0:16].re("p (a b) -> p a b", b=4)
                    self.mm(n_, B["cv"][:, b, kv * 64:(kv + 1) * 64], pc, start=True, stop=False)
                    self.mm(n_, B["sv1"][:, 1, kv * 64:(kv + 1) * 64], nc_, start=False, stop=True)
                    self.mm(d_, onesb, pc, start=True, stop=False)
                    self.mm(d_, onesb[0:TS], nc_, start=False, stop=True)
                r3 = B["rden"][it % 2][:, 0:16].re("p (a b) -> p a b", b=4)
                self.tt(r3, db[:, 0:16].re("p (a b) -> p a b", b=4),
                        self.esink[l][:, 4 * kv:4 * kv + 4].unsq(2).bc([128, 4, 4]), ALU.add)
                self.recip(r3, r3)
                self.tt(B["oswaT"][:, 4 * kv:4 * kv + 4, LS * b:LS * b + LS],
                        nb[:, 0:16].re("p (a b) -> p a b", b=4), r3, ALU.mult)

    def peer(self, kind, t0, TT, l, B):
        P = kind == "p"
        subt = [(i * 128, 128) for i in range(4)] if P else [(0, TS)]
        self.norm_mod(kind, B, TT, l, 2)
        hT = B["hT"]
        if not P:
            self.dma(B["mG"].v, self.load_mod_s(B, l, 5))
        for g in range(4):
            s = self.slot()
            self.wload(s, self.peer_wq, l, g * 512, 512)
            self.proj_fm(s, 512, hT, TT, lambda cc, ps, g=g: self.cp(B["pqT"][:, g * 4 + cc, :], ps,
                                                                     eng="scalar" if cc % 2 else "vector"))
        self.dma(B["pkT"].v, self.pkT_in[l], eng="gpsimd")
        self.memset(B["thT"].v, 0.0, eng="gpsimd")
        ident = self.cfv("ident")
        pm = B["pm"]
        pmb = B["pmb"]
        for si, (c0, n) in enumerate(subt):
            sc = B["sc"]
            for half in range(2):
                ps2 = [self.bank(), self.bank()]
                for q in range(8):
                    hc = half * 8 + q
                    self.mm(ps2[q // 4][0:n, (q % 4) * 128:(q % 4 + 1) * 128], B["pqT"][:, hc, c0:c0 + n],
                            B["pkT"][:, hc, :])
                for j in range(2):
                    self.cp(sc[:, half * 8 + j * 4:half * 8 + j * 4 + 4, :],
                            ps2[j][0:n, :].re("p (a b) -> p a b", a=4), eng="scalar")
            top16 = B["top16"]
            for hc in range(16):
                self.max8(top16[:, hc, 0:8], sc[:, hc, :])
                self.mrep(B["scr"].v, top16[:, hc, 0:8], sc[:, hc, :])
                self.max8(top16[:, hc, 8:16], B["scr"].v)
            c24 = B["c24"]
            cm = B["comb"]
            for h in range(8):
                self.tt(cm[0].v.re("p (a b) -> p a b", a=16), top16[:, 2 * h, :].unsq(2).bc([n, 16, 16]),
                        top16[:, 2 * h + 1, :].unsq(1).bc([n, 16, 16]), ALU.add)
                self.max8(c24[:, h, 0:8], cm[0].v)
                self.mrep(cm[1].v, c24[:, h, 0:8], cm[0].v)
                self.max8(c24[:, h, 8:16], cm[1].v)
                self.mrep(cm[2].v, c24[:, h, 8:16], cm[1].v)
                self.max8(c24[:, h, 16:24], cm[2].v)
            thr, negm, Z, gf = pm[:, 0:8], pm[:, 8:16], pm[:, 16:24], pm[:, 24:32]
            r0, r1, r2 = pm[:, 32:40], pm[:, 40:48], pm[:, 48:56]
            ex = pm[:, 64:192]
            th3 = pm[:, 192:216]
            th3v = th3.re("p (h k) -> p h k", k=3)
            self.tt(thr, c24[:, :, 15], c24[:, :, 16], ALU.add)
            self.ts(thr, thr, 0.5, ALU.mult)
            self.ts(negm, c24[:, :, 0], -1.0, ALU.mult)
            for h in range(8):
                self.act(ex[:, h * 16:(h + 1) * 16], c24[:, h, 0:16], AF.Exp, bias=negm[:, h:h + 1],
                         accum=Z[:, h:h + 1])
            self.tt(gf, thr, negm, ALU.add)
            self.act(gf, gf, AF.Exp)
            self.recip(Z, Z)
            self.tt(gf, gf, Z, ALU.mult)
            self.ts(r0, thr, -1.0, ALU.mult)
            self.cp(pmb[:, 0:8], r0)
            self.tt(r1, r0, pmb[:, 0:8], ALU.subtract)
            self.cp(pmb[:, 8:16], r1)
            self.tt(r2, r1, pmb[:, 8:16], ALU.subtract)
            self.cp(pmb[:, 16:24], r2)
            for kk in range(3):
                self.cp(th3v[:, :, kk], pmb[:, 8 * kk:8 * kk + 8])
            pst = self.bank()
            self.transpose(pst[0:24, 0:n], th3, ident[0:n, 0:n])
            self.cp(B["thT"][0:24, c0:c0 + n], pst[0:24, 0:n], eng="scalar")
            pst2 = self.bank()
            self.transpose(pst2[0:8, 0:n], gf, ident[0:n, 0:n])
            self.cp(B["gfT"][:, c0:c0 + n], pst2[0:8, 0:n], eng="scalar")
        for h in range(8):
            ps = self.bank()
            self.mm(ps[:, 0:TT], ident[0:8, h:h + 1].bc([8, 128]), B["gfT"][:, 0:TT])
            self.cp(B["gbcb"][:, h, :], ps[:, 0:TT], eng="scalar")
        sel24 = self.cbv("sel24")
        NB = 32
        slotU, slotV = self.slots[0], self.slots[1]
        svv = slotV.v.re("p a b -> p (a b)").re("p (a b) -> p a b", a=4)

        def load_u(g):
            self.wload(slotU, self.uT, l, g * 512, 512)

        def load_v(g):
            vsrc = V(self.pv, self.pv.t[l, g * 512:(g + 1) * 512, :].rearrange("(nc p) d -> p nc d", p=128))
            self.dma(svv, vsrc, eng="gpsimd")

        hold = {}

        def a_item(g, i):
            nc_, half = i // 2, i % 2
            hraw = B["hraw"][g % 2]
            if half == 0:
                hold["hid"] = self.bank()
            ps = hold["hid"]
            for kc in range(half * 8, half * 8 + 8):
                self.mm(ps[:, 0:TT], slotU[:, kc, nc_ * 128:(nc_ + 1) * 128], hT[:, kc, 0:TT],
                        start=(kc == 0), stop=(kc == 15))
            if half == 1:
                self.cp(hraw[:, nc_, :], ps[:, 0:TT], eng="scalar")
            if i == 7:
                self.act(hraw.v, hraw.v, AF.Gelu)

        def c_item(g, i):
            AT = B["AT"][g % 2]
            for dch in (2 * i, 2 * i + 1):
                po = self.bank()
                for nc_ in range(4):
                    self.mm(po[:, 0:TT], svv[:, nc_, dch * 128:(dch + 1) * 128], AT[:, nc_, :],
                            start=(nc_ == 0), stop=(nc_ == 3))
                self.residual(kind, B, l, 2, dch, po[:, 0:TT])

        def z_group(g, pair, h):
            ncs = (2 * pair, 2 * pair + 1)
            pzs = []
            for q, nc_ in enumerate(ncs):
                j = g * 4 + nc_
                pz = self.bank()
                self.mm(pz[:, 0:TT], B["pkT"][:, 2 * h + 1, :], B["pqT"][:, 2 * h + 1, :], start=True, stop=False)
                self.mm(pz[:, 0:TT], B["pkT"][:, 2 * h, j:j + 1].bc([128, 128]), B["pqT"][:, 2 * h, :],
                        start=False, stop=False)
                self.mm(pz[:, 0:TT], sel24[:, h:h + 1].bc([128, 128]), B["thT"][:, 0:TT], start=False, stop=True)
                pzs.append(pz)
            for q in range(2):
                self.act(B["zc"][q].v, pzs[q][:, 0:TT], AF.Prelu, alpha=1.0e7)
            e0 = (h % 2) * 2
            for q in range(2):
                self.act(B["E"][e0 + q].v, B["zc"][q].v, AF.Exp)
            if h == 0:
                for q in range(2):
                    self.tt(B["acc"][q].v, B["E"][e0 + q].v, B["gbcb"][:, 0, :], ALU.mult)
            else:
                for q in range(2):
                    self.tt(B["ptmp"][e0 + q].v, B["E"][e0 + q].v, B["gbcb"][:, h, :], ALU.mult)
                for q in range(2):
                    self.tt(B["acc"][q].v, B["acc"][q].v, B["ptmp"][e0 + q].v, ALU.add)

        def finish_pair(g, pair):
            hraw = B["hraw"][g % 2]
            AT = B["AT"][g % 2]
            for q, nc_ in enumerate((2 * pair, 2 * pair + 1)):
                self.tt(AT[:, nc_, :], hraw[:, nc_, :], B["acc"][q].v, ALU.mult)

        load_u(0)
        for i in range(8):
            a_item(0, i)
        load_u(1)
        for g in range(NB):
            if g >= 1:
                load_v(g - 1)
            for h in range(8):
                z_group(g, 0, h)
                if g + 1 < NB:
                    a_item(g + 1, h)
            finish_pair(g, 0)
            if g + 2 < NB:
                load_u(g + 2)
            for h in range(8):
                z_group(g, 1, h)
                if g >= 1:
                    c_item(g - 1, h)
            finish_pair(g, 1)
        load_v(NB - 1)
        for i in range(8):
            c_item(NB - 1, i)
        self.slot_i = 0


_PROG = None


def _prep_inputs(inp):
    f = lambda a: np.ascontiguousarray(np.asarray(a, dtype=np.float32))
    cf, cb = make_consts()
    L = DEPTH
    shared = {
        "w_ada": f(inp["w_ada"]), "w_in": f(inp["w_in"]), "w_out": f(inp["w_out"]), "peer_wq": f(inp["peer_wq"]),
        "uT": f(np.transpose(np.asarray(inp["peer_u"]), (0, 2, 1))),
        "pv": f(inp["peer_v"]),
        "pkT": f(np.transpose(np.asarray(inp["peer_keys"]).reshape(L, 16, 128, 128), (0, 3, 1, 2))),
        "fg": f(np.asarray(inp["final_g"]).reshape(16, 128).T),
        "wal1": f(np.concatenate([np.asarray(inp["w_alpha2"]), np.asarray(inp["b_alpha"])[:, None, :]], axis=1)),
        "cf": cf, "cb": cb,
    }
    pp = np.zeros((L, 128, NPP), np.float32)
    for l in range(L):
        pp[l, :, PP_G1:PP_G1 + 16] = np.asarray(inp["norm1_g"])[l].reshape(16, 128).T
        pp[l, :, PP_G2:PP_G2 + 16] = np.asarray(inp["norm2_g"])[l].reshape(16, 128).T
        pp[l, :, PP_BADA:PP_BADA + 96] = np.asarray(inp["b_ada"])[l].reshape(96, 128).T
        pp[l, :, PP_GNG:PP_GNG + 4] = np.asarray(inp["gla_norm_g"])[l].reshape(4, 128).T
        sk = np.asarray(inp["swa_sinks"])[l]
        pp[l, :, PP_SINK:PP_SINK + 16] = np.repeat(sk.reshape(16, 2).T, 64, axis=0)
    shared["pp"] = pp
    xp = np.asarray(inp["x_prompt"])
    xs = np.asarray(inp["x_sample"])
    cp_ = np.asarray(inp["c_prompt"])
    cs = np.asarray(inp["c_sample"])
    st = np.asarray(inp["state_gla"])
    ck = np.asarray(inp["cache_swa_k"])
    cv = np.asarray(inp["cache_swa_v"])
    maps = []
    for i in range(NCORES):
        sl = slice(NSEQ_S * i, NSEQ_S * (i + 1))
        m = dict(shared)
        m["xT"] = f(np.concatenate([xp[i].T, xs[sl].reshape(TS, D).T], axis=1))
        m["cT"] = f(np.concatenate([cp_[i][None, :], np.repeat(cs[sl], LS, axis=0)], axis=0).T)
        m["state"] = f(st[:, sl])
        cki = ck[:, sl]
        ckt = np.zeros((L, 64, 4, 2048 + 64), np.float32)
        ckt[:, :, :, :2048] = np.transpose(cki, (0, 4, 1, 3, 2)).reshape(L, 64, 4, 2048)
        m["ckT"] = ckt
        m["ck"] = f(cki.reshape(L, NSEQ_S, 128, 256))
        m["cv"] = f(cv[:, sl].reshape(L, NSEQ_S, 128, 256))
        maps.append(m)
    return maps


def _assemble(res):
    L = DEPTH
    yp = np.stack([res[i]["yT"][:, :SEQ].T for i in range(NCORES)])
    ys = np.concatenate([res[i]["yT"][:, SEQ:].T.reshape(NSEQ_S, LS, D) for i in range(NCORES)])
    glap = np.stack([res[i]["gla_p"] for i in range(NCORES)], axis=1)
    kp = np.stack([res[i]["k_p"].reshape(L, 128, 4, 64) for i in range(NCORES)], axis=1)
    vp = np.stack([res[i]["v_p"].reshape(L, 128, 4, 64) for i in range(NCORES)], axis=1)
    glas = np.concatenate([res[i]["gla_s"] for i in range(NCORES)], axis=1)
    ks = np.concatenate([res[i]["k_s"].reshape(L, NSEQ_S, 128, 4, 64) for i in range(NCORES)], axis=1)
    vs = np.concatenate([res[i]["v_s"].reshape(L, NSEQ_S, 128, 4, 64) for i in range(NCORES)], axis=1)
    return tuple(np.ascontiguousarray(a, dtype=np.float32) for a in (yp, ys, glap, kp, vp, glas, ks, vs))


def kernel(**inputs):
    global _PROG
    if _PROG is None:
        _PROG = Prog()
    maps = _prep_inputs(inputs)
    r = run_bass_kernel_spmd(_PROG.nc, maps, core_ids=list(range(NCORES)))
    return _assemble(r.results)
```
